# Optimizing a Trainium2 kernel written in Bass

```python
import jax, jax.numpy as jnp
from jax import lax
import numpy as np

D_MODEL = 2048
BATCH = 4
SEQ = 2048
DEPTH = 1
DEC_BATCH = 128
DEC_SEQ = 8
PAST_LEN = 16384
PAGE_SIZE = 128

POOL_WINDOWS = (2, 4, 8, 16)
POOL_GROUPS = 4
POOL_GROUP_WIDTH = D_MODEL // 8
POOL_WIDTH = POOL_GROUPS * POOL_GROUP_WIDTH
POOL_BUF = 15
MLSTM_HEADS = 4
MLSTM_HEAD_DIM = D_MODEL // 4
MLSTM_WIDTH = MLSTM_HEADS * MLSTM_HEAD_DIM
MLSTM_CHUNK = 128
MEM_TOKENS = 256
MEM_HEADS = 4
MEM_HEAD_DIM = D_MODEL // 8
MEM_WIDTH = MEM_HEADS * MEM_HEAD_DIM
N_BRANCHES = 3
EPS = 1e-6
IN_SIZES = (POOL_WIDTH, POOL_WIDTH,
            MLSTM_WIDTH, MLSTM_WIDTH, MLSTM_WIDTH, MLSTM_WIDTH, MLSTM_WIDTH, MLSTM_HEADS, MLSTM_HEADS,
            MEM_WIDTH, MEM_WIDTH,
            D_MODEL, D_MODEL, D_MODEL)
N_IN = sum(IN_SIZES)

kernel_name = "hybrid_pool_mlstm_memory_decode_step"


def rmsnorm(x, g):
    xf = x.astype(jnp.float32)
    y = xf * lax.rsqrt(jnp.mean(xf * xf, axis=-1, keepdims=True) + EPS) * g.astype(jnp.float32)
    return y.astype(x.dtype)


def pool_mixer(v, buf, start):
    B, L, _ = v.shape
    u = jnp.concatenate([buf.astype(v.dtype), v], axis=1)
    cs = jnp.cumsum(u.astype(jnp.float32), axis=1)
    cs = jnp.concatenate([jnp.zeros((B, 1, POOL_WIDTH), jnp.float32), cs], axis=1)
    pos = start + jnp.arange(L)
    outs = []
    for g, w in enumerate(POOL_WINDOWS):
        sl = slice(g * POOL_GROUP_WIDTH, (g + 1) * POOL_GROUP_WIDTH)
        s = cs[:, POOL_BUF + 1:POOL_BUF + 1 + L, sl] - cs[:, POOL_BUF + 1 - w:POOL_BUF + 1 - w + L, sl]
        cnt = jnp.minimum(w, pos + 1).astype(jnp.float32)
        outs.append(s / cnt[None, :, None])
    p = jnp.concatenate(outs, axis=-1) - v.astype(jnp.float32)
    return p, u[:, -POOL_BUF:]


def mlstm_chunk_step(carry, inp):
    C0, n0, m0 = carry
    q, k, v, ig, lf = inp
    c = q.shape[2]
    b = jnp.cumsum(lf, axis=-1)
    tril = jnp.tril(jnp.ones((c, c), dtype=bool))
    dlog = jnp.where(tril, b[..., :, None] - b[..., None, :] + ig[..., None, :], -jnp.inf)
    inter = b + m0[..., None]
    m = jnp.maximum(inter, jnp.max(dlog, axis=-1))
    dw = jnp.exp(dlog - m[..., None])
    inter_w = jnp.exp(inter - m)
    s = jnp.einsum('bhtd,bhsd->bhts', q, k) * dw
    num = jnp.einsum('bhts,bhsd->bhtd', s, v) + inter_w[..., None] * jnp.einsum('bhvk,bhtk->bhtv', C0, q)
    den = jnp.sum(s, axis=-1) + inter_w * jnp.einsum('bhk,bhtk->bht', n0, q)
    h = num / jnp.maximum(jnp.abs(den), jnp.exp(-m))[..., None]
    m_new = m[..., -1]
    w = jnp.exp(b[..., -1:] - b + ig - m_new[..., None])
    decay = jnp.exp(b[..., -1] + m0 - m_new)
    C_new = decay[..., None, None] * C0 + jnp.einsum('bhs,bhsv,bhsk->bhvk', w, v, k)
    n_new = decay[..., None] * n0 + jnp.einsum('bhs,bhsk->bhk', w, k)
    return (C_new, n_new, m_new), h


def mlstm(q, k, v, i_pre, f_pre, C0, n0, m0):
    B, L, _ = q.shape
    c = min(MLSTM_CHUNK, L)
    nc = L // c

    def heads(t):
        return t.astype(jnp.float32).reshape(B, nc, c, MLSTM_HEADS, MLSTM_HEAD_DIM).transpose(1, 0, 3, 2, 4)

    def gates(t):
        return t.astype(jnp.float32).reshape(B, nc, c, MLSTM_HEADS).transpose(1, 0, 3, 2)

    qh, vh = heads(q), heads(v)
    kh = heads(k) * (MLSTM_HEAD_DIM ** -0.5)
    ih = gates(i_pre)
    lf = jax.nn.log_sigmoid(gates(f_pre))
    carry0 = (C0.astype(jnp.float32), n0.astype(jnp.float32), m0.astype(jnp.float32))
    (C, n, m), hs = lax.scan(mlstm_chunk_step, carry0, (qh, kh, vh, ih, lf))
    h = hs.transpose(1, 0, 3, 2, 4).reshape(B, L, MLSTM_WIDTH)
    return h, C.astype(C0.dtype), n.astype(n0.dtype), m.astype(m0.dtype)


def mem_kv(mem, g_mem, w_mem_kv):
    B = mem.shape[0]
    kv = rmsnorm(mem, g_mem) @ w_mem_kv
    k, v = jnp.split(kv, 2, axis=-1)
    return (k.reshape(B, MEM_TOKENS, MEM_HEADS, MEM_HEAD_DIM),
            v.reshape(B, MEM_TOKENS, MEM_HEADS, MEM_HEAD_DIM))


def cross_attn(q, mem_k, mem_v):
    B, L, _ = q.shape
    qh = q.reshape(B, L, MEM_HEADS, MEM_HEAD_DIM)
    s = jnp.einsum('blhd,bmhd->bhlm', qh, mem_k.astype(q.dtype)).astype(jnp.float32) * (MEM_HEAD_DIM ** -0.5)
    a = jax.nn.softmax(s, axis=-1).astype(q.dtype)
    o = jnp.einsum('bhlm,bmhd->blhd', a, mem_v.astype(q.dtype))
    return o.reshape(B, L, MEM_WIDTH)


def mixer_layer(x, start, pool_buf, C0, n0, m0, mem_k, mem_v, g_pre, g_post, w_in, b_mlstm_i, b_mlstm_f,
                w_pool_grp, pool_scale, w_br_pool, w_br_mlstm, w_br_mem, w_out):
    B, L, _ = x.shape
    dt = x.dtype
    h = rmsnorm(x, g_pre)
    proj = h @ w_in
    idx = np.cumsum(IN_SIZES)[:-1].tolist()
    (pv, pz, q, k, v, o, z, ig, fg, cq, cz, ga, gb, gc) = jnp.split(proj, idx, axis=-1)
    p, new_buf = pool_mixer(pv, pool_buf, start)
    pa = jnp.einsum('blgc,gcd->blgd', p.reshape(B, L, POOL_GROUPS, POOL_GROUP_WIDTH),
                    w_pool_grp.astype(jnp.float32)).reshape(B, L, POOL_WIDTH) * pool_scale.astype(jnp.float32)
    ya = (pa.astype(dt) * jax.nn.silu(pz)) @ w_br_pool
    hb, C, n, m = mlstm(q, k, v, ig + b_mlstm_i, fg + b_mlstm_f, C0, n0, m0)
    yb = (jax.nn.sigmoid(o) * hb.astype(dt) * jax.nn.silu(z)) @ w_br_mlstm
    yc = (cross_attn(cq, mem_k, mem_v) * jax.nn.silu(cz)) @ w_br_mem
    merged = jax.nn.sigmoid(ga) * ya + jax.nn.sigmoid(gb) * yb + jax.nn.sigmoid(gc) * yc
    y = x + rmsnorm(merged @ w_out, g_post)
    return y, new_buf, C, n, m


def setup_inputs(seed: int = 0) -> dict:
    key = jax.random.key(seed)
    ks = jax.random.split(key, 24)
    f32 = jnp.float32
    nrm = lambda k, s, sc=1.0: jax.random.normal(k, s, f32) * sc
    Dp, Db = DEPTH, DEC_BATCH
    return {
        "x_prompt": nrm(ks[0], (BATCH, SEQ, D_MODEL)),
        "x_sample": nrm(ks[1], (DEC_BATCH, DEC_SEQ, D_MODEL)),
        "state_pool": nrm(ks[2], (Dp, Db, POOL_BUF, POOL_WIDTH)),
        "state_mlstm_C": nrm(ks[3], (Dp, Db, MLSTM_HEADS, MLSTM_HEAD_DIM, MLSTM_HEAD_DIM)),
        "state_mlstm_n": nrm(ks[4], (Dp, Db, MLSTM_HEADS, MLSTM_HEAD_DIM)),
        "state_mlstm_m": nrm(ks[5], (Dp, Db, MLSTM_HEADS)),
        "cache_mem_k": nrm(ks[6], (Dp, Db, MEM_TOKENS, MEM_HEADS, MEM_HEAD_DIM)),
        "cache_mem_v": nrm(ks[7], (Dp, Db, MEM_TOKENS, MEM_HEADS, MEM_HEAD_DIM)),
        "mem_prompt": nrm(ks[8], (BATCH, MEM_TOKENS, D_MODEL)),
        "g_pre": 1.0 + nrm(ks[9], (Dp, D_MODEL), 0.1),
        "g_post": 1.0 + nrm(ks[10], (Dp, D_MODEL), 0.1),
        "w_in": nrm(ks[11], (Dp, D_MODEL, N_IN), D_MODEL ** -0.5),
        "b_mlstm_i": nrm(ks[12], (Dp, MLSTM_HEADS), 0.1),
        "b_mlstm_f": jnp.linspace(3.0, 6.0, MLSTM_HEADS, dtype=f32)[None, :] + nrm(ks[13], (Dp, MLSTM_HEADS), 0.1),
        "w_pool_grp": nrm(ks[14], (Dp, POOL_GROUPS, POOL_GROUP_WIDTH, POOL_GROUP_WIDTH), POOL_GROUP_WIDTH ** -0.5),
        "pool_scale": 1.0 + nrm(ks[15], (Dp, POOL_WIDTH), 0.1),
        "g_mem": 1.0 + nrm(ks[16], (Dp, D_MODEL), 0.1),
        "w_mem_kv": nrm(ks[17], (Dp, D_MODEL, 2 * MEM_WIDTH), D_MODEL ** -0.5),
        "w_br_pool": nrm(ks[18], (Dp, POOL_WIDTH, D_MODEL), POOL_WIDTH ** -0.5),
        "w_br_mlstm": nrm(ks[19], (Dp, MLSTM_WIDTH, D_MODEL), MLSTM_WIDTH ** -0.5),
        "w_br_mem": nrm(ks[20], (Dp, MEM_WIDTH, D_MODEL), MEM_WIDTH ** -0.5),
        "w_out": nrm(ks[21], (Dp, D_MODEL, D_MODEL), D_MODEL ** -0.5),
    }


def reference(x_prompt, x_sample, state_pool, state_mlstm_C, state_mlstm_n, state_mlstm_m, cache_mem_k,
              cache_mem_v, mem_prompt, g_pre, g_post, w_in, b_mlstm_i, b_mlstm_f, w_pool_grp, pool_scale,
              g_mem, w_mem_kv, w_br_pool, w_br_mlstm, w_br_mem, w_out):
    xp, xs = x_prompt, x_sample
    Bp = x_prompt.shape[0]
    dt = x_prompt.dtype
    pool_p, C_p, n_p, m_p, mk_p, mv_p = [], [], [], [], [], []
    pool_s, C_s, n_s, m_s = [], [], [], []
    for l in range(DEPTH):
        wl = (g_pre[l], g_post[l], w_in[l], b_mlstm_i[l], b_mlstm_f[l], w_pool_grp[l], pool_scale[l],
              w_br_pool[l], w_br_mlstm[l], w_br_mem[l], w_out[l])
        mk, mv = mem_kv(mem_prompt, g_mem[l], w_mem_kv[l])
        xp, pb, C, n, m = mixer_layer(
            xp, 0,
            jnp.zeros((Bp, POOL_BUF, POOL_WIDTH), dt),
            jnp.zeros((Bp, MLSTM_HEADS, MLSTM_HEAD_DIM, MLSTM_HEAD_DIM), dt),
            jnp.zeros((Bp, MLSTM_HEADS, MLSTM_HEAD_DIM), dt),
            jnp.zeros((Bp, MLSTM_HEADS), dt),
            mk, mv, *wl)
        pool_p.append(pb); C_p.append(C); n_p.append(n); m_p.append(m); mk_p.append(mk); mv_p.append(mv)
        xs, pb, C, n, m = mixer_layer(
            xs, PAST_LEN, state_pool[l], state_mlstm_C[l], state_mlstm_n[l], state_mlstm_m[l],
            cache_mem_k[l], cache_mem_v[l], *wl)
        pool_s.append(pb); C_s.append(C); n_s.append(n); m_s.append(m)
    return (xp, xs,
            jnp.stack(pool_p), jnp.stack(C_p), jnp.stack(n_p), jnp.stack(m_p), jnp.stack(mk_p), jnp.stack(mv_p),
            jnp.stack(pool_s), jnp.stack(C_s), jnp.stack(n_s), jnp.stack(m_s))
```

```python
import numpy as np
from contextlib import ExitStack
import concourse.bass as bass
import concourse.mybir as mybir
from concourse.bass_utils import run_bass_kernel_spmd

F32 = mybir.dt.float32
BF16 = mybir.dt.bfloat16
AF = mybir.ActivationFunctionType
ALU = mybir.AluOpType
AX = mybir.AxisListType
NDS = 8
NF8 = 2
NB4 = 6


class Op:
    __slots__ = ("eng", "fn", "deps", "kind", "ev", "pre", "needed")

    def __init__(self, eng, fn, kind):
        self.eng = eng
        self.fn = fn
        self.deps = []
        self.kind = kind
        self.ev = None
        self.pre = None
        self.needed = False


class _Rec:
    def __init__(self):
        self.calls = []

    def __getattr__(self, name):
        def f(*a, **k):
            self.calls.append((name, a, k))
            return self
        return f


def _bind(fn):
    if fn is None:
        return None
    r = _Rec()
    fn(r)
    calls = r.calls

    def replay(eng):
        ins = None
        for (name, a, k) in calls:
            ins = getattr(eng, name)(*a, **k)
        return ins
    return replay


class Prog:
    CE = ("pe", "act", "dve", "pool")

    def __init__(self, nc, es):
        self.nc = nc
        self.ops = {e: [] for e in ("pe", "act", "dve", "pool", "sp")}
        self.res = {}
        self.csem = {e: es.enter_context(nc.semaphore("c_" + e)) for e in self.CE}
        self.dsem = {q: [es.enter_context(nc.semaphore("d_%s%d" % (q, i))) for i in range(NDS)]
                     for q in ("sp", "pool")}
        self.drr = {q: 0 for q in self.dsem}
        self.dtot = {}

    def _track(self, op, reads, writes):
        psr = [r for r in reads if isinstance(r, str) and r.startswith("ps") and r[2:].isdigit()]
        if psr:
            reads = [r for r in reads if r not in psr]
            writes = list(writes) + psr
        deps = []
        for r in reads:
            st = self.res.get(r)
            if st is not None and st[0] is not None:
                deps.append(st[0])
        for w in writes:
            st = self.res.get(w)
            if st is not None:
                if st[0] is not None:
                    deps.append(st[0])
                deps.extend(st[1])
        seen = set()
        flat = []
        for d in deps:
            if d.kind == "v":
                flat.extend(d.deps)
            else:
                flat.append(d)
        for d in flat:
            if id(d) in seen or d is op:
                continue
            seen.add(id(d))
            if d.kind == "c" and op.kind == "c" and d.eng == "pe" and op.eng == "pe":
                continue
            op.deps.append(d)
            if op.kind != "v":
                d.needed = True
                if d.kind == "v":
                    raise AssertionError("virtual dep leaked")
        for r in reads:
            st = self.res.get(r)
            if st is None:
                self.res[r] = [None, [op]]
            else:
                st[1].append(op)
        for w in writes:
            self.res[w] = [op, []]

    def c(self, eng, fn, reads=(), writes=()):
        op = Op(eng, _bind(fn), "c")
        self._track(op, reads, writes)
        self.ops[eng].append(op)
        return op

    def v(self, writes):
        op = Op("dve", None, "v")
        self._track(op, (), writes)
        return op

    def d(self, q, fn, reads=(), writes=()):
        op = Op(q, _bind(fn), "d")
        i = self.drr[q]
        self.drr[q] = (i + 1) % NDS
        sem = self.dsem[q][i]
        prev = self.dtot.get(sem, 0)
        if prev:
            op.pre = (sem, prev)
        self.dtot[sem] = prev + 16
        op.ev = (sem, prev + 16)
        self._track(op, reads, writes)
        self.ops[q].append(op)
        return op

    def final(self, ops):
        op = Op("sp", None, "c")
        for d in ops:
            op.deps.append(d)
            d.needed = True
        self.ops["sp"].append(op)

    def emit(self):
        nc = self.nc
        for e in self.CE:
            n = 0
            for op in self.ops[e]:
                if op.kind == "c" and op.needed:
                    n += 1
                    op.ev = (self.csem[e], n)
        prog = self

        def run(e, eng):
            waited = {}
            for op in prog.ops[e]:
                ws = []
                if op.pre is not None:
                    ws.append(op.pre)
                for dd in op.deps:
                    ws.append(dd.ev)
                best = {}
                for (s, v) in ws:
                    k = id(s)
                    if v > best.get(k, (None, 0))[1]:
                        best[k] = (s, v)
                for k, (s, v) in best.items():
                    if waited.get(k, 0) < v:
                        eng.wait_ge(s, v)
                        waited[k] = v
                if op.fn is None:
                    continue
                ins = op.fn(eng)
                if op.kind == "d":
                    ins.then_inc(op.ev[0], 16)
                elif op.ev is not None:
                    ins.then_inc(op.ev[0], 1)

        with nc.Block() as block:
            @block.tensor
            def _(eng):
                run("pe", eng)

            @block.scalar
            def _(eng):
                run("act", eng)

            @block.vector
            def _(eng):
                run("dve", eng)

            @block.gpsimd
            def _(eng):
                run("pool", eng)

            @block.sync
            def _(eng):
                run("sp", eng)


D = 2048
KC = 16
NT = 9
NPF = 8
NIN = 20488
OFF = dict(pv=0, pz=1024, q=2048, k=4096, v=6144, o=8192, z=10240, ig=12288, cq=12296, cz=13320,
           ga=14344, gb=16392, gc=18440)
EPS = 1e-6
STOP = None


class _Stop(Exception):
    pass


def _ck(tag):
    if STOP == tag:
        raise _Stop()


def build():
    nc = bass.Bass("TRN2", target_bir_lowering=False)

    def di(n, s):
        return nc.dram_tensor(n, list(s), F32, kind="ExternalInput").ap()

    def do(n, s):
        return nc.dram_tensor(n, list(s), F32, kind="ExternalOutput").ap()

    xm = di("xm", [NT * 128, D]); xp = di("xp", [NPF * 128, D]); memx = di("memx", [256, D])
    spool = di("spool", [16, 15, 1024]); sC = di("sC", [16, 4, 512, 512]); sn = di("sn", [16, 4, 512])
    sm = di("sm", [16, 4]); ck = di("ck", [16, 256, 1024]); cv = di("cv", [16, 256, 1024])
    w_in = di("w_in", [D, NIN]); w_kv = di("w_kv", [D, 2048]); wbp = di("wbp", [1024, D]); wbm = di("wbm", [2048, D])
    wbc = di("wbc", [1024, D]); w_out = di("w_out", [D, D]); wpg_d = di("wpg", [4, 256, 256])
    g_pre = di("g_pre", [1, D]); g_post = di("g_post", [1, D]); g_mem = di("g_mem", [1, D])
    pscale = di("pscale", [1, 1024]); bif = di("bif", [1, 8])
    c_ident = di("c_ident", [128, 128]); c_uf = di("c_uf", [128, 128]); c_ub = di("c_ub", [128, 128])
    c_of = di("c_of", [128, 128]); c_ob = di("c_ob", [128, 128])
    c_band = di("c_band", [128, 16, 128])
    c_bandb = di("c_bandb", [120, 8, 128])
    c_rowm = di("c_rowm", [128, 16]); c_sel8 = di("c_sel8", [128, 16]); c_blk = di("c_blk", [128, 16, 128])
    c_selc = di("c_selc", [128, 1])

    y = do("y", [NT * 128, D]); poolp = do("poolp", [15, 1024]); Cp = do("Cp", [4, 512, 512]); np_ = do("np_", [4, 512])
    mp = do("mp", [1, 4]); mk = do("mk", [256, 1024]); mv = do("mv", [256, 1024])
    pools = do("pools", [16, 15, 1024]); Cs = do("Cs", [16, 4, 512, 512]); ns = do("ns", [16, 4, 512]); ms = do("ms", [16, 4])

    hps = nc.dram_tensor("hps", [NPF, 128, KC * 128], BF16).ap()
    yin = nc.dram_tensor("yin", [NT, 128, 32 * 128], BF16).ap()
    mgd = nc.dram_tensor("mgd", [NT, 128, KC * 128], BF16).ap()
    yps = nc.dram_tensor("yps", [NT, 128, D], F32).ap()

    outs = []
    with ExitStack() as es:
        P = Prog(nc, es)

        def sb(n, s, d):
            return es.enter_context(nc.sbuf_tensor("s_" + n, list(s), d))

        hT = sb("hT", [128, KC, NT * 128], BF16)
        R3 = sb("R3", [128, 4, NT + 1, 512], BF16)
        W = [sb("W%d" % i, [128, KC, 512], BF16) for i in range(2)]
        f8 = [sb("f8_%d" % i, [128, 2048], F32) for i in range(NF8)]
        b4 = [sb("b4_%d" % i, [128, 2048], BF16) for i in range(NB4)]
        gbc = sb("gbc", [128, 2048], F32)
        Cst = gbc[:].rearrange("p (a b) -> p a b", a=4); Cb = sb("Cb", [128, 4, 512], BF16)
        nst = sb("nst", [128, 4], F32); nb = sb("nb", [128, 4], BF16)
        identf = sb("identf", [128, 128], F32); identb = sb("identb", [128, 128], BF16)
        uf = sb("uf", [128, 128], F32); ub = sb("ub", [128, 128], F32)
        of_ = sb("of", [128, 128], F32); ob = sb("ob", [128, 128], F32)
        band = sb("band", [128, 16, 128], BF16); bandb = sb("bandb", [120, 8, 128], BF16)
        rowm = sb("rowm", [128, 16], F32); rowmb = sb("rowmb", [128, 16], BF16); sel8 = sb("sel8", [128, 16], F32)
        blk = band; selc = sb("selc", [128, 1], F32)
        onesb = sb("onesb", [128, 1], BF16)
        wpg = sb("wpg", [128, 8, 256], BF16); psbc = f8[0][:, 0:1024]
        wg = sb("wg", [128, KC, 8], BF16); bifbc = sb("bifbc", [128, 8], F32)
        ARENA = sb("arena", [128, 8192], BF16)
        KTp = ARENA[:, 0:2048].rearrange("p (a b) -> p a b", a=8); Vp = ARENA[:, 2048:4096].rearrange("p (a b) -> p a b", a=2)
        memhT = R3[:, 0, 0:8, :].rearrange("p a (b c) -> p (a b) c", c=256)
        st = sb("st", [128, 3, 8], F32)
        NCH = 17
        G = sb("G", [128, NCH, 8], F32); lf = sb("lf", [128, NCH, 4], F32); t1 = sb("t1", [128, NCH, 4], F32)
        ball = sb("ball", [128, NCH, 4], F32); blast = sb("blast", [128, NCH, 4], F32); aall = sb("aall", [128, NCH, 4], F32)
        aT = sb("aT", [68, 128], F32); amx = sb("amx", [68, 16], F32); amaxbc = sb("amaxbc", [128, NCH, 4], F32)
        m0 = sb("m0", [128, NCH + 1, 4], F32); Mp = sb("Mp", [128, NCH, 4], F32)
        wall = sb("wall", [128, NCH, 4], F32); dall = sb("dall", [128, NCH + 1, 4], F32); thr = sb("thr", [128, NCH, 4], F32)
        tmpg = sb("tmpg", [128, NCH, 4], F32)
        dsall = sb("dsall", [128, 16, 4], F32); dsrc = sb("dsrc", [128, 16, 4], F32); d16 = sb("d16", [16, 4], F32)
        wj = sb("wj", [128, 16], F32)
        n0 = sb("n0", [16, 512], F32); nout = sb("nout", [16, 512], F32)
        small = sb("small", [128, 16], F32)
        msum = sb("msum", [128, NT, 4], F32)

        ps = [es.enter_context(nc.psum_tensor("ps%d" % i, [128, 512], F32)) for i in range(8)]
        psb = [p.bitcast(BF16) for p in ps]

        cnt = {"w": 0, "p": 0, "p3": 0, "q": 0, "f": 0, "b": 0, "t": 0}

        def rr(kind, n):
            i = cnt[kind]
            cnt[kind] = (i + 1) % n
            return i

        def alloc_b():
            bi = rr("b", NB4)
            sub = [("b4h", bi, 0), ("b4h", bi, 1), ("b4p", bi, 0), ("b4p", bi, 8), ("b4a", bi), ("b4t", bi), ("b4m", bi), ("b4v", bi)]
            sub += [("b4q", bi, k) for k in range(4)] + [("b4c", bi, k) for k in range(4)]
            P.v(["b4_%d" % bi] + sub)
            return bi

        def alloc_f():
            fi = rr("f", NF8)
            P.v(["f8_%d" % fi] + [("f8c", fi, k) for k in range(4)])
            return fi

        def ld(q, out_ap, in_ap, writes, reads=()):
            return P.d(q, lambda e, o=out_ap, i=in_ap: e.dma_start(out=o, in_=i), reads=reads, writes=writes)

        NW = [2]

        def load_w(src, k0, nk, c0, ncols):
            s = rr("w", NW[0])
            ld("pool", W[s][:, 0:nk, 0:ncols],
               src[k0 * 128:(k0 + nk) * 128, c0:c0 + ncols].rearrange("(kc p) n -> p kc n", p=128), ["W%d" % s])
            return s

        def mm_group(bank, lhs_list, rhs_list, n, m=128, reads=(), col0=0):
            def fn(e):
                ins = None
                L = len(lhs_list)
                for i in range(L):
                    ins = e.matmul(ps[bank][0:m, col0:col0 + n], lhsT=lhs_list[i], rhs=rhs_list[i],
                                   start=(i == 0), stop=(i == L - 1))
                return ins
            return P.c("pe", fn, reads=list(reads), writes=["ps%d" % bank])

        def transposes(bank, ins_list, idt, reads, bf=True, m=128, col0=0, wkey=None):
            def fn(e):
                ins = None
                c = col0
                for a in ins_list:
                    kk = a.shape[0]
                    mm_ = a.shape[1]
                    tgt = (psb[bank] if bf else ps[bank])[0:mm_, c:c + kk]
                    ins = e.transpose(out=tgt, in_=a, identity=idt[0:kk, 0:kk])
                    c += kk
                return ins
            return P.c("pe", fn, reads=list(reads), writes=["ps%d" % bank])

        try:
            ld("sp", identf[:], c_ident, ["identf"]); ld("pool", identb[:], c_ident, ["identb"])
            ld("sp", uf[:], c_uf, ["uf"]); ld("sp", ub[:], c_ub, ["ub"]); ld("sp", of_[:], c_of, ["of"]); ld("sp", ob[:], c_ob, ["ob"])
            ld("pool", band[:], c_band, ["band"]); ld("pool", bandb[:], c_bandb, ["bandb"])
            ld("sp", rowm[:], c_rowm, ["rowm"]); ld("pool", rowmb[:], c_rowm, ["rowmb"]); ld("sp", sel8[:], c_sel8, ["sel8"])
            ld("sp", selc[:], c_selc, ["selc"])
            ld("sp", psbc, pscale.partition_broadcast(128), ["psbc", "f8_0"])
            ld("sp", bifbc[:], bif.partition_broadcast(128), ["bifbc"])
            ld("pool", wpg[:], wpg_d.rearrange("g (ci p) d -> p (g ci) d", p=128), ["wpg"])
            ld("pool", wg[:], w_in[:, OFF["ig"]:OFF["ig"] + 8].rearrange("(kc p) n -> p kc n", p=128), ["wg"])
            P.c("dve", lambda e: e.memset(onesb[:], 1.0), writes=["onesb"])
            P.c("dve", lambda e: e.tensor_tensor(
                out=wpg[:].rearrange("p (g ci) d -> p g ci d", ci=2), in0=wpg[:].rearrange("p (g ci) d -> p g ci d", ci=2),
                in1=psbc.rearrange("p (g d) -> p g d", g=4).unsqueeze(2).broadcast_to([128, 4, 2, 256]), op=ALU.mult),
                reads=["wpg", "psbc"], writes=["wpg", "f8_0"])

            def norm_tile(src_rows, gkey, dst_fn):
                fi = alloc_f(); bi = alloc_b(); ji = alloc_b(); si = rr("t", 3)
                xt = f8[fi]; hn = b4[bi]; junk = b4[ji]
                fk, bk, jk, sk = "f8_%d" % fi, "b4_%d" % bi, "b4_%d" % ji, "st%d" % si
                ld("sp", xt[:], src_rows, [fk])
                P.c("act", lambda e: e.activation(out=junk[:], in_=xt[:], func=AF.Square, accum_out=st[:, si, 0:1]),
                    reads=[fk], writes=[jk, sk])
                P.c("act", lambda e: e.activation(out=st[:, si, 1:2], in_=st[:, si, 0:1], func=AF.Sqrt, scale=1.0 / D, bias=EPS),
                    reads=[sk], writes=[sk])
                P.c("dve", lambda e: e.reciprocal(out=st[:, si, 2:3], in_=st[:, si, 1:2]), reads=[sk], writes=[sk])
                P.c("dve", lambda e: e.scalar_tensor_tensor(out=hn[:], in0=xt[:], scalar=st[:, si, 2:3], in1=gbc[:],
                                                            op0=ALU.mult, op1=ALU.mult), reads=[fk, sk, gkey], writes=[bk])
                for half in range(2):
                    bank = 4 + rr("q", 2)
                    transposes(bank, [hn[:, (half * 8 + k) * 128:(half * 8 + k + 1) * 128] for k in range(8)], identb,
                               reads=[bk, "identb"])
                    dst, dkeys = dst_fn(half)
                    eng = "act" if half == 0 else "dve"
                    if eng == "act":
                        P.c("act", lambda e, d_=dst, b_=bank: e.copy(out=d_, in_=psb[b_][:, 0:1024].rearrange("p (k t) -> p k t", k=8)),
                            reads=["ps%d" % bank], writes=dkeys)
                    else:
                        P.c("dve", lambda e, d_=dst, b_=bank: e.tensor_copy(out=d_, in_=psb[b_][:, 0:1024].rearrange("p (k t) -> p k t", k=8)),
                            reads=["ps%d" % bank], writes=dkeys)

            ld("sp", gbc[:], g_pre.partition_broadcast(128), ["gbc_pre"])
            kv_slots = [load_w(w_kv, 0, KC, cb_ * 512, 512) for cb_ in range(2)]
            for i in range(NT):
                norm_tile(xm[i * 128:(i + 1) * 128, :], "gbc_pre",
                          lambda half, i=i: (hT[:, half * 8:(half + 1) * 8, i * 128:(i + 1) * 128], [("hT", i, half)]))
            hTk = lambda i: [("hT", i, 0), ("hT", i, 1)]
            for i in range(NPF):
                bi = alloc_b()
                hp = b4[bi]
                norm_tile(xp[i * 128:(i + 1) * 128, :], "gbc_pre",
                          lambda half, hp=hp, bi=bi: (hp[:, half * 1024:(half + 1) * 1024].rearrange("p (k t) -> p k t", k=8),
                                                      [("b4h", bi, half)]))
                P.d("sp", lambda e, hp=hp, i=i: e.dma_start(out=hps[i], in_=hp[:]), reads=[("b4h", bi, 0), ("b4h", bi, 1)],
                    writes=[("hps", i), "b4_%d" % bi])

            def load_hp(i):
                bi = alloc_b()
                P.d("sp", lambda e, bi=bi, i=i: e.dma_start(out=b4[bi][:], in_=hps[i]), reads=[("hps", i)], writes=["b4_%d" % bi])
                return bi

            _ck("A")
            P.d("sp", lambda e: e.dma_start(out=gbc[:], in_=g_mem.partition_broadcast(128)), writes=["gbc_pre", "gbc_mem"])
            for i in range(2):
                norm_tile(memx[i * 128:(i + 1) * 128, :], "gbc_mem",
                          lambda half, i=i: (memhT[:, half * 8:(half + 1) * 8, i * 128:(i + 1) * 128], [("memhT", i, half)]))
            mhk = [("memhT", i, h) for i in range(2) for h in range(2)]
            for cb in range(4):
                s = kv_slots[cb] if cb < 2 else load_w(w_kv, 0, KC, cb * 512, 512)
                for i in range(2):
                    bank = rr("p", 4)
                    mm_group(bank, [memhT[:, k, i * 128:(i + 1) * 128] for k in range(KC)], [W[s][:, k, :] for k in range(KC)], 512,
                             reads=mhk + ["W%d" % s])
                    fi = alloc_f()
                    P.c("act", lambda e, fi=fi, bank=bank: e.copy(out=f8[fi][:, 0:512], in_=ps[bank][:]),
                        reads=["ps%d" % bank], writes=["f8_%d" % fi])
                    dst = (mk if cb < 2 else mv)[i * 128:(i + 1) * 128, (cb % 2) * 512:(cb % 2) * 512 + 512]
                    outs.append(P.d("sp", lambda e, fi=fi, dst=dst: e.dma_start(out=dst, in_=f8[fi][:, 0:512]), reads=["f8_%d" % fi]))
                    if cb >= 2:
                        P.c("dve", lambda e, bank=bank, i=i, cb=cb: e.tensor_copy(out=Vp[:, i, (cb - 2) * 512:(cb - 1) * 512], in_=ps[bank][:]),
                            reads=["ps%d" % bank], writes=[("Vp", i, cb)])
                if cb < 2:
                    for dt_ in range(4):
                        bank = rr("p", 4)
                        mm_group(bank, [W[s][:, k, dt_ * 128:(dt_ + 1) * 128] for k in range(KC)], [memhT[:, k, :] for k in range(KC)], 256,
                                 reads=mhk + ["W%d" % s])
                        P.c("dve", lambda e, bank=bank, dc=cb * 4 + dt_: e.tensor_copy(out=KTp[:, dc, :], in_=ps[bank][:, 0:256]),
                            reads=["ps%d" % bank], writes=[("KTp", cb * 4 + dt_)])
            P.v(mhk + [("R3", 0, i_) for i_ in range(8)])
            KTpk = [("KTp", i) for i in range(8)]
            Vpk = [("Vp", i, cb) for i in range(2) for cb in (2, 3)]

            _ck("B")
            for ch in range(NCH):
                if ch < 8:
                    bi = load_hp(ch)
                    lhs = [b4[bi][:, k * 128:(k + 1) * 128] for k in range(KC)]
                    rds = ["b4_%d" % bi, "wg"]
                else:
                    i = ch - 8
                    lhs = [hT[:, k, i * 128:(i + 1) * 128] for k in range(KC)]
                    rds = hTk(i) + ["wg"]
                bank = rr("p", 4)
                mm_group(bank, lhs, [wg[:, k, :] for k in range(KC)], 8, reads=rds)
                P.c("dve", lambda e, bank=bank, ch=ch: e.tensor_tensor(out=G[:, ch, :], in0=ps[bank][:, 0:8], in1=bifbc[:], op=ALU.add),
                    reads=["ps%d" % bank, "bifbc"], writes=["G"])
            ig = G[:, :, 0:4]; fg = G[:, :, 4:8]
            P.c("act", lambda e: e.activation(out=t1[:], in_=fg, func=AF.Abs), reads=["G"], writes=["t1"])
            P.c("act", lambda e: e.activation(out=t1[:], in_=t1[:], func=AF.Exp, scale=-1.0), reads=["t1"], writes=["t1"])
            P.c("act", lambda e: e.activation(out=t1[:], in_=t1[:], func=AF.Ln, bias=1.0), reads=["t1"], writes=["t1"])
            P.c("dve", lambda e: e.scalar_tensor_tensor(out=lf[:], in0=fg, scalar=0.0, in1=t1[:], op0=ALU.min, op1=ALU.subtract),
                reads=["G", "t1"], writes=["lf"])
            lff = lf[:].rearrange("p c h -> p (c h)")

            def cums(e):
                e.matmul(ps[6][:, 0:64], lhsT=uf[:], rhs=lff[:, 0:64], start=True, stop=True)
                e.matmul(ps[6][:, 64:68], lhsT=ub[:], rhs=lff[:, 64:68], start=True, stop=True)
                e.matmul(ps[6][:, 128:192], lhsT=of_[:], rhs=lff[:, 0:64], start=True, stop=True)
                return e.matmul(ps[6][:, 192:196], lhsT=ob[:], rhs=lff[:, 64:68], start=True, stop=True)
            P.c("pe", cums, reads=["lf", "uf", "ub", "of", "ob"], writes=["ps6"])
            P.c("dve", lambda e: e.tensor_copy(out=ball[:].rearrange("p c h -> p (c h)"), in_=ps[6][:, 0:68]), reads=["ps6"], writes=["ball"])
            P.c("dve", lambda e: e.tensor_copy(out=blast[:].rearrange("p c h -> p (c h)"), in_=ps[6][:, 128:196]), reads=["ps6"], writes=["blast"])
            P.c("dve", lambda e: e.tensor_tensor(out=aall[:], in0=ig, in1=ball[:], op=ALU.subtract), reads=["G", "ball"], writes=["aall"])
            P.c("pe", lambda e: e.transpose(out=ps[7][0:68, 0:128], in_=aall[:].rearrange("p c h -> p (c h)"), identity=identf[:]),
                reads=["aall", "identf"], writes=["ps7"])
            P.c("dve", lambda e: e.tensor_reduce(out=amx[0:64, 0:1], in_=ps[7][0:64, 0:128], axis=AX.X, op=ALU.max), reads=["ps7"], writes=["amx"])
            P.c("dve", lambda e: e.tensor_reduce(out=amx[64:68, 0:16], in_=ps[7][64:68, 0:128].rearrange("p (j i) -> p j i", i=8),
                                                 axis=AX.X, op=ALU.max), reads=["ps7"], writes=["amx2"])
            P.c("dve", lambda e: e.tensor_copy(out=aT[0:64, :], in_=amx[0:64, 0:1].broadcast_to([64, 128])), reads=["amx"], writes=["aT"])
            P.c("dve", lambda e: e.tensor_copy(out=aT[64:68, :].rearrange("p (j i) -> p j i", i=8),
                                               in_=amx[64:68, 0:16].unsqueeze(2).broadcast_to([4, 16, 8])), reads=["amx2"], writes=["aT2"])
            P.c("pe", lambda e: e.transpose(out=ps[7][:, 128:196], in_=aT[:], identity=identf[0:68, 0:68]),
                reads=["aT", "aT2", "identf"], writes=["ps7"])
            P.c("dve", lambda e: e.tensor_copy(out=amaxbc[:].rearrange("p c h -> p (c h)"), in_=ps[7][:, 128:196]), reads=["ps7"], writes=["amaxbc"])
            P.c("dve", lambda e: e.memset(m0[:], 0.0), writes=["m0"])
            ld("sp", m0[:, 16, :], bass.AP(sm.tensor, 0, [[4, 16], [0, 8], [1, 4]]), ["m0s"], reads=["m0"])
            for ch in range(16):
                P.c("dve", lambda e, ch=ch: e.tensor_tensor(out=Mp[:, ch, :], in0=amaxbc[:, ch, :], in1=m0[:, ch, :], op=ALU.max),
                    reads=["amaxbc", "m0"], writes=["Mp"])
                nxt = ch + 1 if ch < 15 else 17
                P.c("dve", lambda e, ch=ch, nxt=nxt: e.tensor_tensor(out=m0[:, nxt, :], in0=blast[:, ch, :], in1=Mp[:, ch, :], op=ALU.add),
                    reads=["blast", "Mp"], writes=["m0"])
                if ch == 7:
                    P.c("dve", lambda e: e.tensor_scalar(out=m0[:, 8, :], in0=m0[:, 8, :], scalar1=selc[:, 0:1], scalar2=None, op0=ALU.mult),
                        reads=["m0", "selc"], writes=["m0"])
            P.c("dve", lambda e: e.tensor_tensor(out=Mp[:, 16, :], in0=amaxbc[:, 16, :], in1=m0[:, 16, :], op=ALU.max),
                reads=["amaxbc", "m0", "m0s"], writes=["Mp"])
            P.c("dve", lambda e: e.tensor_tensor(out=small[:, 0:4], in0=blast[:, 16, :], in1=Mp[:, 16, :], op=ALU.add),
                reads=["blast", "Mp"], writes=["small"])
            outs.append(P.d("sp", lambda e: e.dma_start(out=mp, in_=m0[0:1, 17, :]), reads=["m0"]))
            outs.append(P.d("sp", lambda e: e.dma_start(out=ms, in_=bass.AP(small, 0, [[16 * 8, 16], [1, 4]])), reads=["small"]))
            P.c("dve", lambda e: e.tensor_tensor(out=wall[:], in0=aall[:], in1=Mp[:], op=ALU.subtract), reads=["aall", "Mp"], writes=["wall"])
            P.c("act", lambda e: e.activation(out=wall[:], in_=wall[:], func=AF.Exp), reads=["wall"], writes=["wall"])
            P.c("dve", lambda e: e.tensor_tensor(out=dall[:, 0:NCH, :], in0=m0[:, 0:NCH, :], in1=Mp[:], op=ALU.subtract),
                reads=["m0", "m0s", "Mp"], writes=["dall"])
            P.c("act", lambda e: e.activation(out=dall[:, 0:NCH, :], in_=dall[:, 0:NCH, :], func=AF.Exp), reads=["dall"], writes=["dall"])
            P.c("dve", lambda e: e.memset(dall[:, NCH, :], 1.0), reads=["dall"], writes=["dall"])
            P.c("dve", lambda e: e.tensor_tensor(out=thr[:], in0=ball[:], in1=Mp[:], op=ALU.add), reads=["ball", "Mp"], writes=["thr"])
            P.c("act", lambda e: e.activation(out=thr[:], in_=thr[:], func=AF.Exp, scale=-1.0), reads=["thr"], writes=["thr"])
            P.c("dve", lambda e: e.memset(tmpg[:, 7, :], 1.0), writes=["tmpg"])
            for c_ in range(6, -1, -1):
                P.c("dve", lambda e: e.tensor_tensor(out=tmpg[:, c_, :], in0=tmpg[:, c_ + 1, :], in1=dall[:, c_ + 1, :], op=ALU.mult),
                    reads=["tmpg", "dall"], writes=["tmpg"])
            P.c("dve", lambda e: e.tensor_tensor(out=wall[:, 0:8, :], in0=wall[:, 0:8, :], in1=tmpg[:, 0:8, :], op=ALU.mult),
                reads=["tmpg", "wall"], writes=["wall"])
            P.c("dve", lambda e: e.tensor_tensor(out=dsrc[:], in0=dall[:, 16, :].unsqueeze(1).broadcast_to([128, 16, 4]),
                                                 in1=sel8[:].unsqueeze(2).broadcast_to([128, 16, 4]), op=ALU.mult),
                reads=["dall", "sel8"], writes=["dsrc"])
            P.c("pe", lambda e: e.matmul(ps[6][:, 256:320], lhsT=of_[:], rhs=dsrc[:].rearrange("p j h -> p (j h)"), start=True, stop=True),
                reads=["dsrc", "of"], writes=["ps6"])
            P.c("dve", lambda e: e.tensor_copy(out=dsall[:].rearrange("p j h -> p (j h)"), in_=ps[6][:, 256:320]), reads=["ps6"], writes=["dsall"])
            P.c("pe", lambda e: e.matmul(ps[6][0:16, 320:324], lhsT=sel8[:], rhs=dall[:, 16, :], start=True, stop=True),
                reads=["dall", "sel8", "dsall"], writes=["ps6"])
            P.c("dve", lambda e: e.tensor_copy(out=d16[:], in_=ps[6][0:16, 320:324]), reads=["ps6"], writes=["d16"])
            GK = ["wall", "dall", "thr", "dsall", "d16"]

            _ck("C")
            def proj_block(c0, slot, evac, tiles=range(NT), ncols=512):
                s = load_w(w_in, 0, KC, c0, ncols)
                for i in tiles:
                    bank = rr("p", 4)
                    mm_group(bank, [hT[:, k, i * 128:(i + 1) * 128] for k in range(KC)], [W[s][:, k, 0:ncols] for k in range(KC)], ncols,
                             reads=hTk(i) + ["W%d" % s])
                    evac(i, bank, slot)

            def r3k(slot, i):
                return ("R3", slot, i)

            def ev_copy(eng="act", scale=None):
                def f(i, bank, slot):
                    if eng == "act":
                        if scale is None:
                            P.c("act", lambda e: e.copy(out=R3[:, slot, i, :], in_=ps[bank][:]), reads=["ps%d" % bank], writes=[r3k(slot, i)])
                        else:
                            P.c("act", lambda e: e.mul(out=R3[:, slot, i, :], in_=ps[bank][:], mul=scale), reads=["ps%d" % bank], writes=[r3k(slot, i)])
                    else:
                        P.c("dve", lambda e: e.tensor_copy(out=R3[:, slot, i, :], in_=ps[bank][:]), reads=["ps%d" % bank], writes=[r3k(slot, i)])
                return f

            def ev_act(func):
                def f(i, bank, slot):
                    P.c("act", lambda e: e.activation(out=R3[:, slot, i, :], in_=ps[bank][:], func=func), reads=["ps%d" % bank], writes=[r3k(slot, i)])
                return f

            def to_featmajor(src_aps, src_keys, nk, ydst_kc0, i, extra_reads=()):
                bi = alloc_b()
                for g0 in range(0, nk, 8):
                    n = min(8, nk - g0)
                    bank = 4 + rr("q", 2)
                    transposes(bank, src_aps[g0:g0 + n], identb, reads=list(src_keys) + ["identb"] + list(extra_reads))
                    P.c("act", lambda e, bank=bank, n=n, g0=g0: e.copy(out=b4[bi][:, g0 * 128:(g0 + n) * 128], in_=psb[bank][:, 0:n * 128]),
                        reads=["ps%d" % bank], writes=[("b4p", bi, g0)])
                P.d("sp", lambda e: e.dma_start(out=yin[i][:, ydst_kc0 * 128:(ydst_kc0 + nk) * 128], in_=b4[bi][:, 0:nk * 128]),
                    reads=[("b4p", bi, g0) for g0 in range(0, nk, 8)], writes=[("yin", i, ydst_kc0), "b4_%d" % bi])

            pvprev = ARENA[:, 6144:7168]
            bi7 = load_hp(7)
            for cbk in range(2):
                s = load_w(w_in, 0, KC, OFF["pv"] + cbk * 512, 512)
                bank = rr("p", 4)
                mm_group(bank, [b4[bi7][:, k * 128:(k + 1) * 128] for k in range(KC)], [W[s][:, k, :] for k in range(KC)], 512,
                         reads=["b4_%d" % bi7, "W%d" % s])
                P.c("act", lambda e, bank=bank, cbk=cbk: e.copy(out=pvprev[:, cbk * 512:(cbk + 1) * 512], in_=ps[bank][:]),
                    reads=["ps%d" % bank], writes=[("pvprev", cbk)])

            def ev_pv(i, bank, slot):
                P.c("act", lambda e: e.copy(out=R3[:, slot, i, :], in_=ps[bank][:]), reads=["ps%d" % bank], writes=[r3k(slot, i)])
                if i >= 7:
                    fi = alloc_f()
                    fk = "f8_%d" % fi
                    P.c("dve", lambda e: e.tensor_copy(out=f8[fi][:, 0:512], in_=ps[bank][:]), reads=["ps%d" % bank], writes=[fk])
                    if i == 7:
                        outs.append(P.d("sp", lambda e: e.dma_start(out=poolp[:, slot * 512:(slot + 1) * 512], in_=f8[fi][113:128, 0:512]),
                                        reads=[fk]))
                    else:
                        for j in range(16):
                            outs.append(P.d("sp", lambda e, j=j: e.dma_start(out=pools[j, 7:15, slot * 512:(slot + 1) * 512],
                                                                             in_=f8[fi][j * 8:(j + 1) * 8, 0:512]), reads=[fk]))
            proj_block(OFF["pv"], 0, ev_pv)
            proj_block(OFF["pv"] + 512, 1, ev_pv)
            proj_block(OFF["pz"], 2, ev_act(AF.Silu))
            proj_block(OFF["pz"] + 512, 3, ev_act(AF.Silu))
            outs.append(P.d("sp", lambda e: e.dma_start(out=pools[:, 0:7, :], in_=spool[:, 8:15, :])))
            bufb = ARENA[0:120, 4096:6144].rearrange("p (a b) -> p a b", a=2)
            for c2 in range(2):
                ld("pool", bufb[:, c2, :], spool[c2 * 8:(c2 + 1) * 8].rearrange("j r c -> (j r) c"), [("bufb", c2)])
            for i in range(NT):
                for hb_ in range(2):
                    bank = 6 + hb_

                    def fn(e, i=i, hb_=hb_, bank=bank):
                        ins = None
                        for q4 in range(4):
                            cc = hb_ * 4 + q4
                            g = cc // 2
                            slot, c0 = cc // 4, (cc % 4) * 128
                            out = ps[bank][:, q4 * 128:(q4 + 1) * 128]
                            if i < 8:
                                kind = 0 if i == 0 else 1
                                e.matmul(out, lhsT=R3[:, slot, i, c0:c0 + 128], rhs=band[:, kind * 4 + g, :], start=True, stop=False)
                                prev = pvprev[:, cc * 128:(cc + 1) * 128] if i == 0 else R3[:, slot, i - 1, c0:c0 + 128]
                                ins = e.matmul(out, lhsT=prev, rhs=band[:, 2 * 4 + g, :], start=False, stop=True)
                            else:
                                e.matmul(out, lhsT=R3[:, slot, i, c0:c0 + 128], rhs=band[:, 3 * 4 + g, :], start=True, stop=False)
                                e.matmul(out, lhsT=bufb[:, 0, cc * 128:(cc + 1) * 128], rhs=bandb[:, g * 2 + 0, :], start=False, stop=False)
                                ins = e.matmul(out, lhsT=bufb[:, 1, cc * 128:(cc + 1) * 128], rhs=bandb[:, g * 2 + 1, :], start=False, stop=True)
                        return ins
                    rds = [r3k(0, i), r3k(1, i), "band", "bandb", ("bufb", 0), ("bufb", 1), ("pvprev", 0), ("pvprev", 1)]
                    if 0 < i < 8:
                        rds += [r3k(0, i - 1), r3k(1, i - 1)]
                    P.c("pe", fn, reads=rds, writes=["ps%d" % bank])
                bi = alloc_b()
                pT = b4[bi]
                P.c("act", lambda e, pT=pT: e.copy(out=pT[:, 0:512], in_=ps[6][:]), reads=["ps6"], writes=[("b4p", bi, 0)])
                P.c("dve", lambda e, pT=pT: e.tensor_copy(out=pT[:, 512:1024], in_=ps[7][:]), reads=["ps7"], writes=[("b4p", bi, 8)])
                yb_i = alloc_b()
                yat = b4[yb_i]
                for hb_ in range(2):
                    bank = rr("p", 4)

                    def fn2(e, hb_=hb_, bank=bank, pT=pT):
                        ins = None
                        for gg in range(2):
                            g = hb_ * 2 + gg
                            for ci in range(2):
                                ins = e.matmul(ps[bank][:, gg * 256:(gg + 1) * 256], lhsT=pT[:, (g * 2 + ci) * 128:(g * 2 + ci + 1) * 128],
                                               rhs=wpg[:, g * 2 + ci, :], start=(ci == 0), stop=(ci == 1))
                        return ins
                    P.c("pe", fn2, reads=[("b4p", bi, 0), ("b4p", bi, 8), "wpg"], writes=["ps%d" % bank])
                    P.c("dve", lambda e, bank=bank, hb_=hb_, yat=yat, i=i: e.tensor_tensor(
                        out=yat[:, hb_ * 512:(hb_ + 1) * 512], in0=ps[bank][:], in1=R3[:, 2 + hb_, i, :], op=ALU.mult),
                        reads=["ps%d" % bank, r3k(2 + hb_, i)], writes=[("b4q", yb_i, hb_)])
                P.v(["b4_%d" % bi] + [("b4p", bi, 0), ("b4p", bi, 8)])
                to_featmajor([yat[:, k * 128:(k + 1) * 128] for k in range(8)], [("b4q", yb_i, 0), ("b4q", yb_i, 1)], 8, 0, i)
                P.v(["b4_%d" % yb_i] + [("b4q", yb_i, 0), ("b4q", yb_i, 1)])

            _ck("E")
            proj_block(OFF["cq"], 0, ev_copy("act"))
            proj_block(OFF["cq"] + 512, 1, ev_copy("dve"))
            proj_block(OFF["cz"], 2, ev_act(AF.Silu))
            proj_block(OFF["cz"] + 512, 3, ev_act(AF.Silu))
            P.v([("KTs", 0), ("KTs", 1), "cqT", ("bufb", 0), ("bufb", 1), ("pvprev", 0), ("pvprev", 1)])
            KTs = ARENA[:, 4096:6144].rearrange("p (a b) -> p a b", a=8)
            cqT = ARENA[:, 6144:7168]
            ot = ARENA[:, 7168:8192]
            mx4 = sb("mx4", [128, 3, 8], F32)
            for i in range(NT):
                bank = 4 + rr("q", 2)
                transposes(bank, [R3[:, k // 4, i, (k % 4) * 128:(k % 4 + 1) * 128] for k in range(8)], identb,
                           reads=[r3k(0, i), r3k(1, i), "identb"])
                P.c("act", lambda e: e.copy(out=cqT[:, 0:1024], in_=psb[bank][:, 0:1024]), reads=["ps%d" % bank], writes=["cqT"])
                nsrc = 1 if i < 8 else 16

                def issue_k(j_):
                    bk__ = alloc_b()
                    ld("pool", b4[bk__][:].rearrange("p (m d) -> p m d", m=2), ck[j_].rearrange("(m p) d -> p m d", p=128), ["b4_%d" % bk__])
                    return bk__
                if i == 8:
                    nxt_k = issue_k(0)
                for j in range(nsrc):
                    if i == 8:
                        bk_ = nxt_k
                        bv_ = alloc_b()
                        ld("pool", b4[bv_][:].rearrange("p (m d) -> p m d", m=2), cv[j].rearrange("(m p) d -> p m d", p=128), ["b4_%d" % bv_])
                        Kb = b4[bk_]; Vb = b4[bv_]
                        for half in range(2):
                            bank = 4 + rr("q", 2)
                            transposes(bank, [Kb[:, m * 1024 + (half * 4 + dq) * 128: m * 1024 + (half * 4 + dq + 1) * 128]
                                              for dq in range(4) for m in range(2)], identb, reads=["b4_%d" % bk_, "identb"])
                            if half == 0:
                                P.c("act", lambda e: e.copy(out=KTs[:, 0:4, :], in_=psb[bank][:, 0:1024].rearrange("p (c m) -> p c m", c=4)),
                                    reads=["ps%d" % bank], writes=[("KTs", 0)])
                            else:
                                P.c("dve", lambda e: e.tensor_copy(out=KTs[:, 4:8, :], in_=psb[bank][:, 0:1024].rearrange("p (c m) -> p c m", c=4)),
                                    reads=["ps%d" % bank], writes=[("KTs", 1)])
                        kvk = [("KTs", 0), ("KTs", 1)]
                        vk = ["b4_%d" % bv_]
                        kt_of = lambda h, dc: KTs[:, h * 2 + dc, :]
                        v_of = lambda h, m, Vb=Vb: Vb[:, m * 1024 + h * 256: m * 1024 + (h + 1) * 256]
                    else:
                        kvk = KTpk
                        vk = Vpk
                        kt_of = lambda h, dc: KTp[:, h * 2 + dc, :]
                        v_of = lambda h, m: Vp[:, m, h * 256:(h + 1) * 256]
                    si = rr("t", 3)
                    mk_ = "mx%d" % si
                    fs = alloc_f()
                    sc = f8[fs]
                    sbanks = []
                    for hb_ in range(2):
                        bank = rr("p", 4)
                        sbanks.append(bank)

                        def fsc(e, hb_=hb_, bank=bank, kt_of=kt_of):
                            ins = None
                            for hh in range(2):
                                h = hb_ * 2 + hh
                                for dc in range(2):
                                    ins = e.matmul(ps[bank][:, hh * 256:(hh + 1) * 256], lhsT=cqT[:, (h * 2 + dc) * 128:(h * 2 + dc + 1) * 128],
                                                   rhs=kt_of(h, dc), start=(dc == 0), stop=(dc == 1))
                            return ins
                        P.c("pe", fsc, reads=["cqT"] + kvk, writes=["ps%d" % bank])
                        P.c("dve", lambda e: e.tensor_reduce(out=mx4[:, si, hb_ * 2:hb_ * 2 + 2], in_=ps[bank][:].rearrange("p (h m) -> p h m", h=2),
                                                             axis=AX.X, op=ALU.max), reads=["ps%d" % bank], writes=[(mk_, hb_)])
                        P.c("dve", lambda e: e.tensor_tensor(out=sc[:, hb_ * 512:(hb_ + 1) * 512].rearrange("p (h m) -> p h m", h=2),
                                                             in0=ps[bank][:].rearrange("p (h m) -> p h m", h=2),
                                                             in1=mx4[:, si, hb_ * 2:hb_ * 2 + 2].unsqueeze(2).broadcast_to([128, 2, 256]), op=ALU.subtract),
                            reads=["ps%d" % bank, (mk_, hb_)], writes=[("f8c", fs, hb_)])
                    ba = alloc_b()
                    ea = b4[ba]
                    P.c("act", lambda e: e.activation(out=ea[:, 0:1024], in_=sc[:, 0:1024], func=AF.Exp, scale=1.0 / 16.0),
                        reads=[("f8c", fs, 0), ("f8c", fs, 1)], writes=["b4_%d" % ba])
                    P.c("dve", lambda e: e.tensor_reduce(out=mx4[:, si, 4:8], in_=ea[:, 0:1024].rearrange("p (h m) -> p h m", h=4), axis=AX.X, op=ALU.add),
                        reads=["b4_%d" % ba], writes=[(mk_, 2)])
                    P.c("dve", lambda e: e.reciprocal(out=mx4[:, si, 4:8], in_=mx4[:, si, 4:8]), reads=[(mk_, 2)], writes=[(mk_, 2)])
                    if i == 8:
                        P.c("dve", lambda e: e.tensor_scalar(out=mx4[:, si, 4:8], in0=mx4[:, si, 4:8], scalar1=rowm[:, j:j + 1], scalar2=None, op0=ALU.mult),
                            reads=[(mk_, 2), "rowm"], writes=[(mk_, 2)])
                    P.c("dve", lambda e: e.tensor_tensor(out=ea[:, 1024:2048].rearrange("p (h m) -> p h m", h=4),
                                                         in0=ea[:, 0:1024].rearrange("p (h m) -> p h m", h=4),
                                                         in1=mx4[:, si, 4:8].unsqueeze(2).broadcast_to([128, 4, 256]), op=ALU.mult),
                        reads=["b4_%d" % ba, (mk_, 2)], writes=[("b4a", ba)])
                    bank = 4 + rr("q", 2)
                    transposes(bank, [ea[:, 1024 + k * 128:1024 + (k + 1) * 128] for k in range(8)], identb, reads=[("b4a", ba), "identb"])
                    bt_ = alloc_b()
                    aTt = b4[bt_]
                    P.c("act", lambda e: e.copy(out=aTt[:, 0:1024], in_=psb[bank][:, 0:1024]), reads=["ps%d" % bank], writes=["b4_%d" % bt_])
                    if i == 8 and j + 1 < nsrc:
                        nxt_k = issue_k(j + 1)
                    for hb_ in range(2):
                        def fo(e, hb_=hb_, v_of=v_of, aTt=aTt, j=j, nsrc=nsrc):
                            ins = None
                            for hh in range(2):
                                h = hb_ * 2 + hh
                                for m in range(2):
                                    ins = e.matmul(ps[6 + hb_][:, hh * 256:(hh + 1) * 256], lhsT=aTt[:, (h * 2 + m) * 128:(h * 2 + m + 1) * 128],
                                                   rhs=v_of(h, m), start=(j == 0 and hh == 0 and m == 0),
                                                   stop=(j == nsrc - 1 and hh == 1 and m == 1), skip_group_check=True)
                            return ins
                        P.c("pe", fo, reads=["b4_%d" % bt_] + vk, writes=["ps%d" % (6 + hb_)])
                for hb_ in range(2):
                    P.c("dve", lambda e: e.tensor_tensor(out=ot[:, hb_ * 512:(hb_ + 1) * 512], in0=ps[6 + hb_][:], in1=R3[:, 2 + hb_, i, :], op=ALU.mult),
                        reads=["ps%d" % (6 + hb_), r3k(2 + hb_, i)], writes=[("ot", hb_)])
                to_featmajor([ot[:, k * 128:(k + 1) * 128] for k in range(8)], [("ot", 0), ("ot", 1)], 8, 24, i)

            _ck("G")
            SC = 512.0 ** -0.5
            ld("pool", band[:], c_blk, ["band", "blk"])
            kp = sb("kp", [128, 512], BF16)
            PT = sb("PT", [128, 128], BF16)
            qk = sb("qkT", [128, 1024], BF16)
            rr_ = sb("rr_", [128, 4], F32)
            qk_s = sb("qk_s", [128, 1024], BF16)
            PT_s = sb("PT_s", [128, 128], BF16)
            kp_s = sb("kp_s", [128, 512], BF16)
            rr_s = sb("rr_s", [128, 4], F32)
            pending = []
            posts = {}

            def make_sample(h):
                si_ = 8 + (h % 2)
                ch = 16
                stt = {}

                def issue_c0(j_):
                    fi_ = alloc_f()
                    ld("sp", f8[fi_][:].rearrange("p (vc k) -> p vc k", vc=4), sC[j_, h].rearrange("(vc p) k -> p vc k", p=128), ["f8_%d" % fi_])
                    return fi_

                def pre():
                    bank = 4 + rr("q", 2)
                    transposes(bank, [R3[:, 0, si_, c * 128:(c + 1) * 128] for c in range(4)] + [R3[:, 1, si_, c * 128:(c + 1) * 128] for c in range(4)],
                               identb, reads=[r3k(0, si_), r3k(1, si_), "identb"])
                    P.c("act", lambda e: e.copy(out=qk_s[:, 0:1024], in_=psb[bank][:, 0:1024]), reads=["ps%d" % bank], writes=["qk_s"])
                    bank = rr("p", 4)
                    mm_group(bank, [qk_s[:, 512 + c * 128:512 + (c + 1) * 128] for c in range(4)], [qk_s[:, c * 128:(c + 1) * 128] for c in range(4)], 128,
                             reads=["qk_s"])
                    P.c("dve", lambda e: e.scalar_tensor_tensor(out=PT_s[:], in0=ps[bank][:, 0:128], scalar=wall[:, ch, h:h + 1],
                                                                in1=ub[:], op0=ALU.mult, op1=ALU.mult),
                        reads=["ps%d" % bank, "wall", "ub"], writes=["PT_s"])
                    P.c("dve", lambda e: e.tensor_scalar(out=kp_s[:], in0=R3[:, 1, si_, :], scalar1=wall[:, ch, h:h + 1], scalar2=None, op0=ALU.mult),
                        reads=[r3k(1, si_), "wall"], writes=["kp_s"])
                    ld("sp", n0[:], sn[:, h, :], ["n0"])
                    fe = alloc_f()
                    n0e = f8[fe][:, 0:512]
                    ld("sp", n0e, bass.AP(sn.tensor, h * 512, [[2048, 16], [0, 8], [1, 512]]), ["f8_%d" % fe])
                    bj = alloc_b()
                    P.c("dve", lambda e: e.scalar_tensor_tensor(out=b4[bj][:, 0:512], in0=R3[:, 0, si_, :], scalar=dall[:, ch, h:h + 1],
                                                                in1=n0e, op0=ALU.mult, op1=ALU.mult, accum_out=rr_s[:, 2:3]),
                        reads=[r3k(0, si_), "dall", "f8_%d" % fe], writes=["b4_%d" % bj, "rrs2"])
                    stt["nxt"] = issue_c0(0)

                def unit(j):
                    fi = stt["nxt"]
                    if j + 1 < 16:
                        stt["nxt"] = issue_c0(j + 1)
                    bc_ = alloc_b(); bct = alloc_b()
                    C0f = f8[fi]; C0b = b4[bc_]; C0T = b4[bct]
                    fk = "f8_%d" % fi
                    P.c("act", lambda e: e.copy(out=C0b[:], in_=C0f[:]), reads=[fk], writes=["b4_%d" % bc_])
                    for kc in range(4):
                        bank = 4 + rr("q", 2)
                        transposes(bank, [C0b[:, vc * 512 + kc * 128: vc * 512 + (kc + 1) * 128] for vc in range(4)], identb,
                                   reads=["b4_%d" % bc_, "identb"])
                        P.c("act", lambda e: e.activation(out=C0T[:, kc * 512:(kc + 1) * 512], in_=psb[bank][:, 0:512],
                                                          func=AF.Copy, scale=dsall[:, j, h:h + 1]),
                            reads=["ps%d" % bank, "dsall"], writes=[("b4c", bct, kc)])
                    bm = alloc_b()
                    qm = b4[bm]
                    P.c("dve", lambda e: e.tensor_tensor(out=qm[:, 0:512].rearrange("p (c t) -> p c t", c=4),
                                                         in0=qk_s[:, 0:512].rearrange("p (c t) -> p c t", c=4),
                                                         in1=blk[:, j, :].unsqueeze(1).broadcast_to([128, 4, 128]), op=ALU.mult),
                        reads=["qk_s", "blk"], writes=["b4_%d" % bm])

                    def fnum(e):
                        ins = None
                        if j == 0:
                            e.matmul(ps[7][:], lhsT=PT_s[:], rhs=R3[:, 2, si_, :], start=True, stop=False)
                        for c in range(4):
                            ins = e.matmul(ps[7][:], lhsT=qm[:, c * 128:(c + 1) * 128], rhs=C0T[:, c * 512:(c + 1) * 512],
                                           start=False, stop=(j == 15 and c == 3))
                        return ins
                    P.c("pe", fnum, reads=["PT_s", r3k(2, si_), "b4_%d" % bm] + [("b4c", bct, kc) for kc in range(4)], writes=["ps7"])
                    P.c("dve", lambda e: e.tensor_tensor(out=wj[:, 0:1], in0=wall[:, ch, h:h + 1], in1=rowm[:, j:j + 1], op=ALU.mult),
                        reads=["wall", "rowm"], writes=["wj"])
                    bkj = alloc_b()
                    P.c("dve", lambda e: e.tensor_scalar(out=b4[bkj][:, 0:512], in0=R3[:, 1, si_, :], scalar1=wj[:, 0:1], scalar2=None,
                                                         op0=ALU.mult), reads=[r3k(1, si_), "wj"], writes=["b4_%d" % bkj])
                    for vc in range(4):
                        bank = rr("p", 4)
                        mm_group(bank, [R3[:, 2, si_, vc * 128:(vc + 1) * 128]], [b4[bkj][:, 0:512]], 512, reads=[r3k(2, si_), "b4_%d" % bkj])
                        P.c("dve", lambda e: e.scalar_tensor_tensor(
                            out=C0f[:, vc * 512:(vc + 1) * 512], in0=C0f[:, vc * 512:(vc + 1) * 512], scalar=dsall[:, j, h:h + 1], in1=ps[bank][:],
                            op0=ALU.mult, op1=ALU.add), reads=["ps%d" % bank, fk, "dsall", "b4_%d" % bc_], writes=[("f8c", fi, vc)])
                    outs.append(P.d("sp", lambda e: e.dma_start(out=Cs[j, h].rearrange("(vc p) k -> p vc k", p=128),
                                                               in_=C0f[:].rearrange("p (vc k) -> p vc k", vc=4)),
                                    reads=[("f8c", fi, vc) for vc in range(4)]))

                def post():
                    bank = rr("p", 4)
                    mm_group(bank, [rowmb[:]], [kp_s[:]], 512, m=16, reads=["rowmb", "kp_s"])
                    P.c("dve", lambda e: e.scalar_tensor_tensor(out=nout[:], in0=n0[:], scalar=d16[:, h:h + 1], in1=ps[bank][0:16, :],
                                                                op0=ALU.mult, op1=ALU.add), reads=["ps%d" % bank, "n0", "d16"], writes=["nout"])
                    outs.append(P.d("sp", lambda e: e.dma_start(out=ns[:, h, :], in_=nout[:]), reads=["nout"], writes=["nout_d"]))
                    P.v(["nout"] + ["nout_d"])
                    P.c("pe", lambda e: e.matmul(ps[6][:, 410:411], lhsT=PT_s[:], rhs=onesb[:], start=True, stop=True), reads=["PT_s", "onesb"], writes=["ps6"])
                    P.c("dve", lambda e: e.tensor_tensor(out=rr_s[:, 0:1], in0=ps[6][:, 410:411], in1=rr_s[:, 2:3], op=ALU.add),
                        reads=["ps6", "rrs2"], writes=["rrs"])
                    P.c("act", lambda e: e.activation(out=rr_s[:, 3:4], in_=rr_s[:, 0:1], func=AF.Abs), reads=["rrs"], writes=["rrs3"])
                    P.c("dve", lambda e: e.tensor_tensor(out=rr_s[:, 0:1], in0=rr_s[:, 3:4], in1=thr[:, ch, h:h + 1], op=ALU.max),
                        reads=["rrs3", "thr"], writes=["rrs"])
                    P.c("dve", lambda e: e.reciprocal(out=rr_s[:, 1:2], in_=rr_s[:, 0:1]), reads=["rrs"], writes=["rrs1"])
                    by = alloc_b()
                    P.c("dve", lambda e: e.scalar_tensor_tensor(out=b4[by][:, 0:512], in0=ps[7][:], scalar=rr_s[:, 1:2],
                                                                in1=R3[:, 3, si_, :], op0=ALU.mult, op1=ALU.mult),
                        reads=["ps7", "rrs1", r3k(3, si_)], writes=[("b4q", by, 0)])
                    to_featmajor([b4[by][:, c * 128:(c + 1) * 128] for c in range(4)], [("b4q", by, 0)], 4, 8 + h * 4, 8)
                return pre, unit, post

            tick = [0]

            def hook():
                tick[0] += 1
                if tick[0] % 3 == 0 and pending:
                    pending.pop(0)()

            for h in range(4):
                ri = lambda i, h=h: i if i < 8 else 8 + (h % 2)
                P.c("dve", lambda e: e.memset(Cst, 0.0), reads=["Cb"], writes=["Cst", "gbc_pre", "gbc_mem"])
                P.c("dve", lambda e: e.memset(Cb[:], 0.0), writes=["Cb"])
                P.c("dve", lambda e: e.memset(nst[:], 0.0), reads=["nb"], writes=["nst"])
                P.c("dve", lambda e: e.memset(nb[:], 0.0), writes=["nb"])
                sk_ = load_w(w_in, 0, KC, OFF["k"] + h * 512, 512)
                sv_ = load_w(w_in, 0, KC, OFF["v"] + h * 512, 512)

                def state_update(ch, kp_ap, kp_keys, v_ap, v_keys, last):
                    for kc in range(4):
                        bank = rr("p", 4)
                        mm_group(bank, [kp_ap[:, kc * 128:(kc + 1) * 128]], [v_ap], 512, reads=list(kp_keys) + list(v_keys))
                        P.c("dve", lambda e, bank=bank, kc=kc: e.scalar_tensor_tensor(
                            out=Cst[:, kc, :], in0=Cst[:, kc, :], scalar=dall[:, ch, h:h + 1], in1=ps[bank][:], op0=ALU.mult, op1=ALU.add),
                            reads=["ps%d" % bank, "Cst", "dall"], writes=["Cst"])

                    def fn(e):
                        ins = None
                        for kc in range(4):
                            ins = e.matmul(ps[6][:, 400 + kc:401 + kc], lhsT=kp_ap[:, kc * 128:(kc + 1) * 128], rhs=onesb[:], start=True, stop=True)
                        return ins
                    P.c("pe", fn, reads=list(kp_keys) + ["onesb"], writes=["ps6"])
                    P.c("dve", lambda e: e.scalar_tensor_tensor(out=nst[:], in0=nst[:], scalar=dall[:, ch, h:h + 1], in1=ps[6][:, 400:404],
                                                                op0=ALU.mult, op1=ALU.add), reads=["ps6", "nst", "dall"], writes=["nst"])
                    if not last:
                        nxt = ch + 1
                        P.c("act", lambda e: e.activation(out=Cb[:], in_=Cst[:], func=AF.Copy, scale=dall[:, nxt, h:h + 1]),
                            reads=["Cst", "dall"], writes=["Cb"])
                        P.c("act", lambda e: e.activation(out=nb[:], in_=nst[:], func=AF.Copy, scale=dall[:, nxt, h:h + 1]),
                            reads=["nst", "dall"], writes=["nb"])

                for ch in range(8):
                    bi = load_hp(ch)
                    lhs = [b4[bi][:, k * 128:(k + 1) * 128] for k in range(KC)]
                    bank = rr("p3", 3)
                    mm_group(bank, lhs, [W[sk_][:, k, :] for k in range(KC)], 512, reads=["b4_%d" % bi, "W%d" % sk_])
                    bk2 = alloc_b()
                    kpp = b4[bk2][:, 0:512]
                    P.c("dve", lambda e: e.tensor_scalar(out=kpp, in0=ps[bank][:], scalar1=wall[:, ch, h:h + 1], scalar2=SC,
                                                         op0=ALU.mult, op1=ALU.mult), reads=["ps%d" % bank, "wall"], writes=["b4_%d" % bk2])
                    bank = rr("p3", 3)
                    mm_group(bank, lhs, [W[sv_][:, k, :] for k in range(KC)], 512, reads=["b4_%d" % bi, "W%d" % sv_])
                    bv = alloc_b()
                    vpp = b4[bv][:, 0:512]
                    P.c("act", lambda e: e.copy(out=vpp, in_=ps[bank][:]), reads=["ps%d" % bank], writes=["b4_%d" % bv])

                    def facc(e, kpp=kpp, vpp=vpp, ch=ch):
                        ins = None
                        for kc in range(4):
                            e.matmul(ps[4 + kc][:], lhsT=kpp[:, kc * 128:(kc + 1) * 128], rhs=vpp, start=(ch == 0), stop=(ch == 7))
                        for kc in range(4):
                            ins = e.matmul(ps[3][:, kc:kc + 1], lhsT=kpp[:, kc * 128:(kc + 1) * 128], rhs=onesb[:],
                                           start=(ch == 0 and kc == 0), stop=(ch == 7 and kc == 3), skip_group_check=True)
                        return ins
                    P.c("pe", facc, reads=["b4_%d" % bk2, "b4_%d" % bv, "onesb"], writes=["ps3", "ps4", "ps5", "ps6", "ps7"])
                for kc in range(4):
                    if kc % 2 == 0:
                        P.c("dve", lambda e: e.tensor_copy(out=Cst[:, kc, :], in_=ps[4 + kc][:]), reads=["ps%d" % (4 + kc)], writes=["Cst"])
                    else:
                        P.c("act", lambda e: e.copy(out=Cst[:, kc, :], in_=ps[4 + kc][:]), reads=["ps%d" % (4 + kc)], writes=["Cst"])
                P.c("dve", lambda e: e.tensor_copy(out=nst[:], in_=ps[3][:, 0:4]), reads=["ps3"], writes=["nst"])
                P.c("act", lambda e: e.activation(out=Cb[:], in_=Cst[:], func=AF.Copy, scale=dall[:, 8, h:h + 1]), reads=["Cst", "dall"], writes=["Cb"])
                P.c("act", lambda e: e.activation(out=nb[:], in_=nst[:], func=AF.Copy, scale=dall[:, 8, h:h + 1]), reads=["nst", "dall"], writes=["nb"])
                _ck("F%dp" % h)
                for i in range(NT):
                    bank = rr("p", 4)
                    mm_group(bank, [hT[:, k, i * 128:(i + 1) * 128] for k in range(KC)], [W[sk_][:, k, :] for k in range(KC)], 512,
                             reads=hTk(i) + ["W%d" % sk_])
                    ev_copy("act", SC)(ri(i), bank, 1)
                    hook()
                for i in range(NT):
                    bank = rr("p", 4)
                    mm_group(bank, [hT[:, k, i * 128:(i + 1) * 128] for k in range(KC)], [W[sv_][:, k, :] for k in range(KC)], 512,
                             reads=hTk(i) + ["W%d" % sv_])
                    ev_copy("dve")(ri(i), bank, 2)
                    hook()

                def wrap(ev):
                    def f(i, bank, slot):
                        ev(ri(i), bank, slot)
                        hook()
                    return f
                proj_block(OFF["q"] + h * 512, 0, wrap(ev_copy("dve")))
                proj_block(OFF["o"] + h * 512, 3, wrap(ev_act(AF.Sigmoid)))

                def ev_z(i, bank, slot):
                    bz = alloc_b()
                    P.c("act", lambda e: e.activation(out=b4[bz][:, 0:512], in_=ps[bank][:], func=AF.Silu), reads=["ps%d" % bank], writes=["b4_%d" % bz])
                    P.c("dve", lambda e: e.tensor_tensor(out=R3[:, 3, i, :], in0=R3[:, 3, i, :], in1=b4[bz][:, 0:512], op=ALU.mult),
                        reads=["b4_%d" % bz, r3k(3, i)], writes=[r3k(3, i)])
                proj_block(OFF["z"] + h * 512, 3, wrap(ev_z))

                _ck("F%dj" % h)
                while pending:
                    pending.pop(0)()
                if h > 0:
                    posts[h - 1]()
                if h == 3:
                    pre_, unit_, post_ = make_sample(h)
                    pre_()
                    for j_ in range(16):
                        pending.append(lambda j_=j_, unit_=unit_: unit_(j_))
                for i in range(8):
                    ch = 8 + i
                    bt = 92
                    bank = 4 + rr("q", 2)
                    transposes(bank, [R3[:, 0, i, c * 128:(c + 1) * 128] for c in range(4)] + [R3[:, 1, i, c * 128:(c + 1) * 128] for c in range(4)],
                               identb, reads=[r3k(0, i), r3k(1, i), "identb"])
                    P.c("act", lambda e, bank=bank, qk=qk: e.copy(out=qk[:, 0:1024], in_=psb[bank][:, 0:1024]), reads=["ps%d" % bank], writes=["b4_%d" % bt])
                    qT = lambda c, qk=qk: qk[:, c * 128:(c + 1) * 128]
                    kT = lambda c, qk=qk: qk[:, 512 + c * 128:512 + (c + 1) * 128]
                    bank = rr("p", 4)
                    mm_group(bank, [kT(c) for c in range(4)], [qT(c) for c in range(4)], 128, reads=["b4_%d" % bt])
                    P.c("dve", lambda e: e.scalar_tensor_tensor(out=PT[:], in0=ps[bank][:, 0:128], scalar=wall[:, ch, h:h + 1],
                                                                in1=uf[:], op0=ALU.mult, op1=ALU.mult),
                        reads=["ps%d" % bank, "wall", "uf"], writes=["PT"])
                    P.c("dve", lambda e: e.tensor_scalar(out=kp[:], in0=R3[:, 1, i, :], scalar1=wall[:, ch, h:h + 1], scalar2=None, op0=ALU.mult),
                        reads=[r3k(1, i), "wall"], writes=["kp"])
                    nbank = rr("p", 4)
                    mm_group(nbank, [PT[:]] + [qT(c) for c in range(4)], [R3[:, 2, i, :]] + [Cb[:, c, :] for c in range(4)], 512,
                             reads=["PT", r3k(2, i), "b4_%d" % bt, "Cb"])

                    def fden(e, qT=qT):
                        e.matmul(ps[6][:, 410:411], lhsT=PT[:], rhs=onesb[:], start=True, stop=False)
                        ins = None
                        for c in range(4):
                            ins = e.matmul(ps[6][:, 410:411], lhsT=qT(c), rhs=nb[:, c:c + 1], start=False, stop=(c == 3))
                        return ins
                    P.c("pe", fden, reads=["PT", "onesb", "b4_%d" % bt, "nb"], writes=["ps6"])
                    P.c("act", lambda e: e.activation(out=rr_[:, 3:4], in_=ps[6][:, 410:411], func=AF.Abs), reads=["ps6"], writes=["rr3"])
                    P.c("dve", lambda e: e.tensor_tensor(out=rr_[:, 0:1], in0=rr_[:, 3:4], in1=thr[:, ch, h:h + 1], op=ALU.max),
                        reads=["rr3", "thr"], writes=["rr_"])
                    P.c("dve", lambda e: e.reciprocal(out=rr_[:, 1:2], in_=rr_[:, 0:1]), reads=["rr_"], writes=["rr1"])
                    by = alloc_b()
                    P.c("dve", lambda e: e.scalar_tensor_tensor(out=b4[by][:, 0:512], in0=ps[nbank][:], scalar=rr_[:, 1:2],
                                                                in1=R3[:, 3, i, :], op0=ALU.mult, op1=ALU.mult),
                        reads=["ps%d" % nbank, "rr1", r3k(3, i)], writes=[("b4q", by, 0)])
                    to_featmajor([b4[by][:, c * 128:(c + 1) * 128] for c in range(4)], [("b4q", by, 0)], 4, 8 + h * 4, i)
                    state_update(ch, kp, ["kp"], R3[:, 2, i, :], [r3k(2, i)], i == 7)
                    if h == 3:
                        for _ in range(2):
                            if pending:
                                pending.pop(0)()
                if h == 3:
                    while pending:
                        pending.pop(0)()
                    post_()
                _ck("F%dm" % h)
                _ck("F%ds" % h)
                for vc in range(4):
                    bank = rr("p", 4)
                    transposes(bank, [Cst[:, kc, vc * 128:(vc + 1) * 128] for kc in range(4)], identf, reads=["Cst", "identf"], bf=False)
                    fi = alloc_f()
                    P.c("act", lambda e, bank=bank, fi=fi: e.copy(out=f8[fi][:, 0:512], in_=ps[bank][:]), reads=["ps%d" % bank], writes=["f8_%d" % fi])
                    outs.append(P.d("sp", lambda e, fi=fi, vc=vc: e.dma_start(out=Cp[h, vc * 128:(vc + 1) * 128, :], in_=f8[fi][:, 0:512]),
                                    reads=["f8_%d" % fi]))
                P.c("pe", lambda e: e.transpose(out=ps[6][0:4, 0:128], in_=nst[:], identity=identf[:]), reads=["nst", "identf"], writes=["ps6"])
                P.c("dve", lambda e: e.tensor_copy(out=small[0:4, 8:8 + 0 + 1].broadcast_to([4, 1]) if False else nout[0:4, 0:128], in_=ps[6][0:4, 0:128]),
                    reads=["ps6"], writes=["nout"])
                outs.append(P.d("sp", lambda e: e.dma_start(out=np_[h].rearrange("(kc k) -> kc k", kc=4), in_=nout[0:4, 0:128]), reads=["nout"], writes=["nout_d"]))
                P.v(["nout"] + ["nout_d"])
                if h < 3:
                    pre_, unit_, post_ = make_sample(h)
                    pre_()
                    posts[h] = post_
                    for j_ in range(16):
                        pending.append(lambda j_=j_, unit_=unit_: unit_(j_))

            _ck("F")
            R3f_ = R3[:].rearrange("p a i c -> p (a i c)")
            W.append(R3f_[:, 10240:10240 + 8192].rearrange("p (k n) -> p k n", k=KC))
            W.append(ARENA[:].rearrange("p (k n) -> p k n", k=KC))
            P.v(["W2"] + [r3k(a_, i_) for a_ in (2, 3) for i_ in range(NT + 1)])
            P.v(["W3", "cqT", ("ot", 0), ("ot", 1), ("KTs", 0), ("KTs", 1)] + KTpk + Vpk)
            NW[0] = 4
            MG = R3[:, 0:2, :, :].rearrange("p a i c -> p (a i c)").bitcast(F32)[:, 0:NT * 512].rearrange("p (i c) -> p i c", c=512)
            P.v([r3k(a_, i_) for a_ in range(4) for i_ in range(NT + 1)] + [("MG", i_) for i_ in range(NT)])
            br = [(wbp, 0, 8, "ga"), (wbm, 8, 16, "gb"), (wbc, 24, 8, "gc")]
            for db in range(4):
                for bidx, (wsrc, kc0, nk, gname) in enumerate(br):
                    sw = load_w(wsrc, 0, nk, db * 512, 512)
                    sg = load_w(w_in, 0, KC, OFF[gname] + db * 512, 512)
                    for i in range(NT):
                        bi = alloc_b()
                        yt = b4[bi]
                        P.d("sp", lambda e, yt=yt, i=i, kc0=kc0, nk=nk: e.dma_start(out=yt[:, 0:nk * 128], in_=yin[i][:, kc0 * 128:(kc0 + nk) * 128]),
                            reads=[("yin", i, kc0 + a) for a in (range(0, nk, 4) if bidx == 1 else [0])], writes=["b4_%d" % bi])
                        gbank = rr("p", 4)
                        mm_group(gbank, [hT[:, k, i * 128:(i + 1) * 128] for k in range(KC)], [W[sg][:, k, :] for k in range(KC)], 512,
                                 reads=hTk(i) + ["W%d" % sg])
                        bs = alloc_b()
                        P.c("act", lambda e, gbank=gbank, bs=bs: e.activation(out=b4[bs][:, 0:512], in_=ps[gbank][:], func=AF.Sigmoid),
                            reads=["ps%d" % gbank], writes=["b4_%d" % bs])
                        ybank = rr("p", 4)
                        mm_group(ybank, [yt[:, k * 128:(k + 1) * 128] for k in range(nk)], [W[sw][:, k, :] for k in range(nk)], 512,
                                 reads=["b4_%d" % bi, "W%d" % sw])
                        if bidx == 0:
                            P.c("dve", lambda e, ybank=ybank, bs=bs, i=i: e.tensor_tensor(out=MG[:, i, :], in0=ps[ybank][:], in1=b4[bs][:, 0:512], op=ALU.mult),
                                reads=["ps%d" % ybank, "b4_%d" % bs], writes=[("MG", i)])
                        else:
                            bt2 = alloc_b()
                            P.c("dve", lambda e, ybank=ybank, bs=bs, bt2=bt2: e.tensor_tensor(out=b4[bt2][:, 0:1024].bitcast(F32), in0=ps[ybank][:],
                                                                                             in1=b4[bs][:, 0:512], op=ALU.mult),
                                reads=["ps%d" % ybank, "b4_%d" % bs], writes=["b4_%d" % bt2])
                            P.c("dve", lambda e, bt2=bt2, i=i: e.tensor_tensor(out=MG[:, i, :], in0=MG[:, i, :], in1=b4[bt2][:, 0:1024].bitcast(F32), op=ALU.add),
                                reads=["b4_%d" % bt2, ("MG", i)], writes=[("MG", i)])
                for i in range(NT):
                    bm = alloc_b()
                    P.c("act", lambda e, bm=bm, i=i: e.copy(out=b4[bm][:, 0:512], in_=MG[:, i, :]), reads=[("MG", i)], writes=["b4_%d" % bm])
                    bank = 4 + rr("q", 2)
                    transposes(bank, [b4[bm][:, c * 128:(c + 1) * 128] for c in range(4)], identb, reads=["b4_%d" % bm, "identb"])
                    P.c("act", lambda e, bank=bank, bm=bm: e.copy(out=b4[bm][:, 512:1024], in_=psb[bank][:, 0:512]), reads=["ps%d" % bank], writes=[("b4m", bm)])
                    P.d("sp", lambda e, bm=bm, i=i, db=db: e.dma_start(out=mgd[i][:, db * 512:(db + 1) * 512], in_=b4[bm][:, 512:1024]),
                        reads=[("b4m", bm)], writes=[("mgd", i, db), "b4_%d" % bm])

            _ck("H")
            P.d("sp", lambda e: e.dma_start(out=gbc[:], in_=g_post.partition_broadcast(128)), writes=["gbc_pre", "gbc_mem", "gbc_post", "Cst"])
            R3f = R3[:].rearrange("p a i c -> p (a i c)")
            wo = [W[0], W[1], R3f[:, 10240:10240 + 8192].rearrange("p (k n) -> p k n", k=KC), None]
            wok = [["W0"], ["W1"], ["W2"], ["R3lo"]]
            P.v(["W2"] + [r3k(a_, i_) for a_ in (2, 3) for i_ in range(NT + 1)])
            ld("pool", wo[2], w_out[:, 1024:1536].rearrange("(kc p) n -> p kc n", p=128), ["W2"])
            P.v(["R3lo"] + [r3k(a_, i_) for a_ in (0, 1) for i_ in range(NT + 1)] + [("MG", i_) for i_ in range(NT)])
            wo[3] = R3f[:, 0:8192].rearrange("p (k n) -> p k n", k=KC)
            ld("pool", wo[3], w_out[:, 1536:2048].rearrange("(kc p) n -> p kc n", p=128), ["R3lo"])
            for cb in range(2):
                ld("pool", W[cb][:], w_out[:, cb * 512:(cb + 1) * 512].rearrange("(kc p) n -> p kc n", p=128), ["W%d" % cb])
            P.v(["W3"])
            def issue_i(i_):
                bi_ = alloc_b()
                P.d("sp", lambda e: e.dma_start(out=b4[bi_][:], in_=mgd[i_]), reads=[("mgd", i_, d_) for d_ in range(4)], writes=["b4_%d" % bi_])
                fx_ = alloc_f()
                P.d("sp", lambda e: e.dma_start(out=f8[fx_][:], in_=xm[i_ * 128:(i_ + 1) * 128, :]), writes=["f8_%d" % fx_])
                return bi_, fx_
            nxt_i = issue_i(0)
            for i in range(NT):
                bi, fx = nxt_i
                xr = f8[fx]
                bj = alloc_b()
                si = rr("t", 3)
                sk = "st%d" % si
                banks = [(i % 2) * 4 + cb for cb in range(4)]
                for cb in range(4):
                    mm_group(banks[cb], [b4[bi][:, k * 128:(k + 1) * 128] for k in range(KC)], [wo[cb][:, k, :] for k in range(KC)], 512,
                             reads=["b4_%d" % bi] + wok[cb])
                    P.c("act", lambda e: e.activation(out=b4[bj][:, 0:512], in_=ps[banks[cb]][:], func=AF.Square, accum_out=msum[:, i, cb:cb + 1]),
                        reads=["ps%d" % banks[cb]], writes=["b4_%d" % bj, ("msum", i, cb)])
                P.c("dve", lambda e: e.tensor_reduce(out=st[:, si, 0:1], in_=msum[:, i, :], axis=AX.X, op=ALU.add),
                    reads=[("msum", i, c) for c in range(4)], writes=[sk])
                P.c("act", lambda e: e.activation(out=st[:, si, 1:2], in_=st[:, si, 0:1], func=AF.Sqrt, scale=1.0 / D, bias=EPS), reads=[sk], writes=[sk])
                P.c("dve", lambda e: e.reciprocal(out=st[:, si, 2:3], in_=st[:, si, 1:2]), reads=[sk], writes=[sk])
                for hf in range(2):
                    bt_ = alloc_b()
                    tmp = b4[bt_][:].bitcast(F32)
                    for q2 in range(2):
                        cb = hf * 2 + q2
                        P.c("dve", lambda e: e.tensor_tensor(out=tmp[:, q2 * 512:(q2 + 1) * 512], in0=ps[banks[cb]][:], in1=gbc[:, cb * 512:(cb + 1) * 512],
                                                             op=ALU.mult), reads=["ps%d" % banks[cb], "gbc_post"], writes=[("b4q", bt_, q2)])
                    P.c("dve", lambda e: e.scalar_tensor_tensor(out=xr[:, hf * 1024:(hf + 1) * 1024], in0=tmp, scalar=st[:, si, 2:3],
                                                                in1=xr[:, hf * 1024:(hf + 1) * 1024], op0=ALU.mult, op1=ALU.add),
                        reads=[("b4q", bt_, 0), ("b4q", bt_, 1), sk, "f8_%d" % fx], writes=[("f8c", fx, hf)])
                if i + 1 < NT:
                    nxt_i = issue_i(i + 1)
                outs.append(P.d("sp", lambda e: e.dma_start(out=y[i * 128:(i + 1) * 128, :], in_=xr[:]), reads=[("f8c", fx, 0), ("f8c", fx, 1)]))
        except _Stop:
            pass
        P.final(outs)
        P.emit()
    return nc


def _consts(s):
    t = np.arange(128)
    S, T = np.meshgrid(t, t, indexing="ij")
    uf = (S <= T).astype(np.float32)
    same = (S // 8 == T // 8)
    ub = (uf * same).astype(np.float32)
    of_ = np.ones((128, 128), np.float32)
    ob = same.astype(np.float32)
    band = np.zeros((128, 16, 128), np.float32)
    bandb = np.zeros((120, 8, 128), np.float32)
    eye = np.eye(128, dtype=np.float32)
    for g, w in enumerate((2, 4, 8, 16)):
        cur = ((S <= T) & (S > T - w)).astype(np.float32) / w - eye
        cnt = np.minimum(w, T + 1).astype(np.float32)
        cur0 = ((S <= T) & (S > T - w)).astype(np.float32) / cnt - eye
        band[:, 0 * 4 + g, :] = cur0 if s == 0 else cur
        band[:, 1 * 4 + g, :] = cur
        band[:, 2 * 4 + g, :] = ((S - 128) > (T - w)).astype(np.float32) / w
        band[:, 3 * 4 + g, :] = (same & (S <= T) & (S > T - w)).astype(np.float32) / w - eye
        r = np.arange(120)
        R, TT = np.meshgrid(r, t, indexing="ij")
        for c2 in range(2):
            jj = c2 * 8 + R // 15
            ii = R % 15
            bandb[:, g * 2 + c2, :] = ((jj == TT // 8) & ((ii - 15) > (TT % 8 - w))).astype(np.float32) / w
    rowm = (t[:, None] // 8 == np.arange(16)[None, :]).astype(np.float32)
    sel8 = (t[:, None] == 8 * np.arange(16)[None, :]).astype(np.float32)
    blk = np.broadcast_to(rowm.T[None, :, :], (128, 16, 128)).astype(np.float32).copy()
    selc = np.full((128, 1), float(s), np.float32)
    return dict(c_ident=eye, c_uf=uf, c_ub=ub, c_of=of_, c_ob=ob, c_band=band, c_bandb=bandb, c_rowm=rowm, c_sel8=sel8,
                c_blk=blk, c_selc=selc)


def make_in_maps(x_prompt, x_sample, state_pool, state_mlstm_C, state_mlstm_n, state_mlstm_m, cache_mem_k, cache_mem_v, mem_prompt,
                 g_pre, g_post, w_in, b_mlstm_i, b_mlstm_f, w_pool_grp, pool_scale, g_mem, w_mem_kv, w_br_pool, w_br_mlstm, w_br_mem, w_out,
                 cores=range(8)):
    f = lambda a: np.ascontiguousarray(np.asarray(a, dtype=np.float32))
    x_prompt, x_sample = f(x_prompt), f(x_sample)
    shared = dict(w_in=f(w_in[0]), w_kv=f(w_mem_kv[0]), wbp=f(w_br_pool[0]), wbm=f(w_br_mlstm[0]), wbc=f(w_br_mem[0]), w_out=f(w_out[0]),
                  wpg=f(w_pool_grp[0]), g_pre=f(g_pre), g_post=f(g_post), g_mem=f(g_mem), pscale=f(pool_scale),
                  bif=f(np.concatenate([np.asarray(b_mlstm_i), np.asarray(b_mlstm_f)], axis=1)))
    in_maps = []
    for c in cores:
        b, s = c // 2, c % 2
        sl = slice(16 * c, 16 * c + 16)
        xm = np.concatenate([x_prompt[b, 1024 * s:1024 * (s + 1)], x_sample[sl].reshape(128, D)], axis=0)
        xp = x_prompt[b, 0:1024] if s == 1 else np.zeros((1024, D), np.float32)
        m = dict(shared)
        m.update(xm=f(xm), xp=f(xp), memx=f(mem_prompt[b]), spool=f(state_pool[0, sl]), sC=f(state_mlstm_C[0, sl]),
                 sn=f(state_mlstm_n[0, sl]), sm=f(state_mlstm_m[0, sl]), ck=f(np.asarray(cache_mem_k)[0, sl].reshape(16, 256, 1024)),
                 cv=f(np.asarray(cache_mem_v)[0, sl].reshape(16, 256, 1024)))
        m.update(_consts(s))
        in_maps.append(m)
    return in_maps


_NC = None


def kernel(x_prompt, x_sample, state_pool, state_mlstm_C, state_mlstm_n, state_mlstm_m, cache_mem_k, cache_mem_v, mem_prompt,
           g_pre, g_post, w_in, b_mlstm_i, b_mlstm_f, w_pool_grp, pool_scale, g_mem, w_mem_kv, w_br_pool, w_br_mlstm, w_br_mem, w_out):
    global _NC
    in_maps = make_in_maps(**locals())
    if _NC is None:
        _NC = build()
    res = run_bass_kernel_spmd(_NC, in_maps, core_ids=list(range(8))).results
    y_p = np.zeros((4, 2048, D), np.float32); y_s = np.zeros((128, 8, D), np.float32)
    pool_p = np.zeros((1, 4, 15, 1024), np.float32); C_p = np.zeros((1, 4, 4, 512, 512), np.float32)
    n_p = np.zeros((1, 4, 4, 512), np.float32); m_p = np.zeros((1, 4, 4), np.float32)
    mk_p = np.zeros((1, 4, 256, 4, 256), np.float32); mv_p = np.zeros((1, 4, 256, 4, 256), np.float32)
    pool_s = np.zeros((1, 128, 15, 1024), np.float32); C_s = np.zeros((1, 128, 4, 512, 512), np.float32)
    n_s = np.zeros((1, 128, 4, 512), np.float32); m_s = np.zeros((1, 128, 4), np.float32)
    for c in range(8):
        b, s = c // 2, c % 2
        sl = slice(16 * c, 16 * c + 16)
        r = res[c]
        y_p[b, 1024 * s:1024 * (s + 1)] = r["y"][0:1024]
        y_s[sl] = r["y"][1024:1152].reshape(16, 8, D)
        pool_s[0, sl] = r["pools"]; C_s[0, sl] = r["Cs"]; n_s[0, sl] = r["ns"]; m_s[0, sl] = r["ms"]
        if s == 1:
            pool_p[0, b] = r["poolp"]; C_p[0, b] = r["Cp"]; n_p[0, b] = r["np_"]; m_p[0, b] = r["mp"][0]
        else:
            mk_p[0, b] = r["mk"].reshape(256, 4, 256); mv_p[0, b] = r["mv"].reshape(256, 4, 256)
    return (y_p, y_s, pool_p, C_p, n_p, m_p, mk_p, mv_p, pool_s, C_s, n_s, m_s)
```

```python
import numpy as np
from contextlib import ExitStack
import concourse.bass as bass
import concourse.mybir as mybir
from concourse.bass_utils import run_bass_kernel_spmd

F32 = mybir.dt.float32
BF16 = mybir.dt.bfloat16
AF = mybir.ActivationFunctionType
ALU = mybir.AluOpType
AX = mybir.AxisListType
NDS = 8
NF8 = 2
NB4 = 6


class Op:
    __slots__ = ("eng", "fn", "deps", "kind", "ev", "pre", "needed")

    def __init__(self, eng, fn, kind):
        self.eng = eng
        self.fn = fn
        self.deps = []
        self.kind = kind
        self.ev = None
        self.pre = None
        self.needed = False


class _Rec:
    def __init__(self):
        self.calls = []

    def __getattr__(self, name):
        def f(*a, **k):
            self.calls.append((name, a, k))
            return self
        return f


def _bind(fn):
    if fn is None:
        return None
    r = _Rec()
    fn(r)
    calls = r.calls

    def replay(eng):
        ins = None
        for (name, a, k) in calls:
            ins = getattr(eng, name)(*a, **k)
        return ins
    return replay


class Prog:
    CE = ("pe", "act", "dve", "pool")

    def __init__(self, nc, es):
        self.nc = nc
        self.ops = {e: [] for e in ("pe", "act", "dve", "pool", "sp")}
        self.res = {}
        self.csem = {e: es.enter_context(nc.semaphore("c_" + e)) for e in self.CE}
        self.dsem = {q: [es.enter_context(nc.semaphore("d_%s%d" % (q, i))) for i in range(NDS)]
                     for q in ("sp", "pool")}
        self.drr = {q: 0 for q in self.dsem}
        self.dtot = {}

    def _track(self, op, reads, writes):
        psr = [r for r in reads if isinstance(r, str) and r.startswith("ps") and r[2:].isdigit()]
        if psr:
            reads = [r for r in reads if r not in psr]
            writes = list(writes) + psr
        deps = []
        for r in reads:
            st = self.res.get(r)
            if st is not None and st[0] is not None:
                deps.append(st[0])
        for w in writes:
            st = self.res.get(w)
            if st is not None:
                if st[0] is not None:
                    deps.append(st[0])
                deps.extend(st[1])
        seen = set()
        flat = []
        for d in deps:
            if d.kind == "v":
                flat.extend(d.deps)
            else:
                flat.append(d)
        for d in flat:
            if id(d) in seen or d is op:
                continue
            seen.add(id(d))
            if d.kind == "c" and op.kind == "c" and d.eng == "pe" and op.eng == "pe":
                continue
            op.deps.append(d)
            if op.kind != "v":
                d.needed = True
                if d.kind == "v":
                    raise AssertionError("virtual dep leaked")
        for r in reads:
            st = self.res.get(r)
            if st is None:
                self.res[r] = [None, [op]]
            else:
                st[1].append(op)
        for w in writes:
            self.res[w] = [op, []]

    def c(self, eng, fn, reads=(), writes=()):
        op = Op(eng, _bind(fn), "c")
        self._track(op, reads, writes)
        self.ops[eng].append(op)
        return op

    def v(self, writes):
        op = Op("dve", None, "v")
        self._track(op, (), writes)
        return op

    def d(self, q, fn, reads=(), writes=()):
        op = Op(q, _bind(fn), "d")
        i = self.drr[q]
        self.drr[q] = (i + 1) % NDS
        sem = self.dsem[q][i]
        prev = self.dtot.get(sem, 0)
        if prev:
            op.pre = (sem, prev)
        self.dtot[sem] = prev + 16
        op.ev = (sem, prev + 16)
        self._track(op, reads, writes)
        self.ops[q].append(op)
        return op

    def final(self, ops):
        op = Op("sp", None, "c")
        for d in ops:
            op.deps.append(d)
            d.needed = True
        self.ops["sp"].append(op)

    def emit(self):
        nc = self.nc
        for e in self.CE:
            n = 0
            for op in self.ops[e]:
                if op.kind == "c" and op.needed:
                    n += 1
                    op.ev = (self.csem[e], n)
        prog = self

        def run(e, eng):
            waited = {}
            for op in prog.ops[e]:
                ws = []
                if op.pre is not None:
                    ws.append(op.pre)
                for dd in op.deps:
                    ws.append(dd.ev)
                best = {}
                for (s, v) in ws:
                    k = id(s)
                    if v > best.get(k, (None, 0))[1]:
                        best[k] = (s, v)
                for k, (s, v) in best.items():
                    if waited.get(k, 0) < v:
                        eng.wait_ge(s, v)
                        waited[k] = v
                if op.fn is None:
                    continue
                ins = op.fn(eng)
                if op.kind == "d":
                    ins.then_inc(op.ev[0], 16)
                elif op.ev is not None:
                    ins.then_inc(op.ev[0], 1)

        with nc.Block() as block:
            @block.tensor
            def _(eng):
                run("pe", eng)

            @block.scalar
            def _(eng):
                run("act", eng)

            @block.vector
            def _(eng):
                run("dve", eng)

            @block.gpsimd
            def _(eng):
                run("pool", eng)

            @block.sync
            def _(eng):
                run("sp", eng)


D = 2048
KC = 16
NT = 9
NPF = 8
NIN = 20488
OFF = dict(pv=0, pz=1024, q=2048, k=4096, v=6144, o=8192, z=10240, ig=12288, cq=12296, cz=13320,
           ga=14344, gb=16392, gc=18440)
EPS = 1e-6
STOP = None


class _Stop(Exception):
    pass


def _ck(tag):
    if STOP == tag:
        raise _Stop()


def build():
    nc = bass.Bass("TRN2", target_bir_lowering=False)

    def di(n, s):
        return nc.dram_tensor(n, list(s), F32, kind="ExternalInput").ap()

    def do(n, s):
        return nc.dram_tensor(n, list(s), F32, kind="ExternalOutput").ap()

    xm = di("xm", [NT * 128, D]); xp = di("xp", [NPF * 128, D]); memx = di("memx", [256, D])
    spool = di("spool", [16, 15, 1024]); sC = di("sC", [16, 4, 512, 512]); sn = di("sn", [16, 4, 512])
    sm = di("sm", [16, 4]); ck = di("ck", [16, 256, 1024]); cv = di("cv", [16, 256, 1024])
    w_in = di("w_in", [D, NIN]); w_kv = di("w_kv", [D, 2048]); wbp = di("wbp", [1024, D]); wbm = di("wbm", [2048, D])
    wbc = di("wbc", [1024, D]); w_out = di("w_out", [D, D]); wpg_d = di("wpg", [4, 256, 256])
    g_pre = di("g_pre", [1, D]); g_post = di("g_post", [1, D]); g_mem = di("g_mem", [1, D])
    pscale = di("pscale", [1, 1024]); bif = di("bif", [1, 8])
    c_ident = di("c_ident", [128, 128]); c_uf = di("c_uf", [128, 128]); c_ub = di("c_ub", [128, 128])
    c_of = di("c_of", [128, 128]); c_ob = di("c_ob", [128, 128])
    c_band = di("c_band", [128, 16, 128])
    c_bandb = di("c_bandb", [120, 8, 128])
    c_rowm = di("c_rowm", [128, 16]); c_sel8 = di("c_sel8", [128, 16]); c_blk = di("c_blk", [128, 16, 128])
    c_selc = di("c_selc", [128, 1])

    y = do("y", [NT * 128, D]); poolp = do("poolp", [15, 1024]); Cp = do("Cp", [4, 512, 512]); np_ = do("np_", [4, 512])
    mp = do("mp", [1, 4]); mk = do("mk", [256, 1024]); mv = do("mv", [256, 1024])
    pools = do("pools", [16, 15, 1024]); Cs = do("Cs", [16, 4, 512, 512]); ns = do("ns", [16, 4, 512]); ms = do("ms", [16, 4])

    hps = nc.dram_tensor("hps", [NPF, 128, KC * 128], BF16).ap()
    yin = nc.dram_tensor("yin", [NT, 128, 32 * 128], BF16).ap()
    mgd = nc.dram_tensor("mgd", [NT, 128, KC * 128], BF16).ap()
    yps = nc.dram_tensor("yps", [NT, 128, D], F32).ap()

    outs = []
    with ExitStack() as es:
        P = Prog(nc, es)

        def sb(n, s, d):
            return es.enter_context(nc.sbuf_tensor("s_" + n, list(s), d))

        hT = sb("hT", [128, KC, NT * 128], BF16)
        R3 = sb("R3", [128, 4, NT + 1, 512], BF16)
        W = [sb("W%d" % i, [128, KC, 512], BF16) for i in range(2)]
        f8 = [sb("f8_%d" % i, [128, 2048], F32) for i in range(NF8)]
        b4 = [sb("b4_%d" % i, [128, 2048], BF16) for i in range(NB4)]
        gbc = sb("gbc", [128, 2048], F32)
        Cst = gbc[:].rearrange("p (a b) -> p a b", a=4); Cb = sb("Cb", [128, 4, 512], BF16)
        nst = sb("nst", [128, 4], F32); nb = sb("nb", [128, 4], BF16)
        identf = sb("identf", [128, 128], F32); identb = sb("identb", [128, 128], BF16)
        uf = sb("uf", [128, 128], F32); ub = sb("ub", [128, 128], F32)
        of_ = sb("of", [128, 128], F32); ob = sb("ob", [128, 128], F32)
        band = sb("band", [128, 16, 128], BF16); bandb = sb("bandb", [120, 8, 128], BF16)
        rowm = sb("rowm", [128, 16], F32); rowmb = sb("rowmb", [128, 16], BF16); sel8 = sb("sel8", [128, 16], F32)
        blk = band; selc = sb("selc", [128, 1], F32)
        onesb = sb("onesb", [128, 1], BF16)
        wpg = sb("wpg", [128, 8, 256], BF16); psbc = f8[0][:, 0:1024]
        wg = sb("wg", [128, KC, 8], BF16); bifbc = sb("bifbc", [128, 8], F32)
        ARENA = sb("arena", [128, 8192], BF16)
        KTp = ARENA[:, 0:2048].rearrange("p (a b) -> p a b", a=8); Vp = ARENA[:, 2048:4096].rearrange("p (a b) -> p a b", a=2)
        memhT = R3[:, 0, 0:8, :].rearrange("p a (b c) -> p (a b) c", c=256)
        st = sb("st", [128, 3, 8], F32)
        NCH = 17
        G = sb("G", [128, NCH, 8], F32); lf = sb("lf", [128, NCH, 4], F32); t1 = sb("t1", [128, NCH, 4], F32)
        ball = sb("ball", [128, NCH, 4], F32); blast = sb("blast", [128, NCH, 4], F32); aall = sb("aall", [128, NCH, 4], F32)
        aT = sb("aT", [68, 128], F32); amx = sb("amx", [68, 16], F32); amaxbc = sb("amaxbc", [128, NCH, 4], F32)
        m0 = sb("m0", [128, NCH + 1, 4], F32); Mp = sb("Mp", [128, NCH, 4], F32)
        wall = sb("wall", [128, NCH, 4], F32); dall = sb("dall", [128, NCH + 1, 4], F32); thr = sb("thr", [128, NCH, 4], F32)
        tmpg = sb("tmpg", [128, NCH, 4], F32)
        dsall = sb("dsall", [128, 16, 4], F32); dsrc = sb("dsrc", [128, 16, 4], F32); d16 = sb("d16", [16, 4], F32)
        wj = sb("wj", [128, 16], F32)
        n0 = sb("n0", [16, 512], F32); nout = sb("nout", [16, 512], F32)
        small = sb("small", [128, 16], F32)
        msum = sb("msum", [128, NT, 4], F32)

        ps = [es.enter_context(nc.psum_tensor("ps%d" % i, [128, 512], F32)) for i in range(8)]
        psb = [p.bitcast(BF16) for p in ps]

        cnt = {"w": 0, "p": 0, "p3": 0, "q": 0, "f": 0, "b": 0, "t": 0}

        def rr(kind, n):
            i = cnt[kind]
            cnt[kind] = (i + 1) % n
            return i

        def alloc_b():
            bi = rr("b", NB4)
            sub = [("b4h", bi, 0), ("b4h", bi, 1), ("b4p", bi, 0), ("b4p", bi, 8), ("b4a", bi), ("b4t", bi), ("b4m", bi), ("b4v", bi)]
            sub += [("b4q", bi, k) for k in range(4)] + [("b4c", bi, k) for k in range(4)]
            P.v(["b4_%d" % bi] + sub)
            return bi

        def alloc_f():
            fi = rr("f", NF8)
            P.v(["f8_%d" % fi] + [("f8c", fi, k) for k in range(4)])
            return fi

        def ld(q, out_ap, in_ap, writes, reads=()):
            return P.d(q, lambda e, o=out_ap, i=in_ap: e.dma_start(out=o, in_=i), reads=reads, writes=writes)

        NW = [2]

        def load_w(src, k0, nk, c0, ncols):
            s = rr("w", NW[0])
            ld("pool", W[s][:, 0:nk, 0:ncols],
               src[k0 * 128:(k0 + nk) * 128, c0:c0 + ncols].rearrange("(kc p) n -> p kc n", p=128), ["W%d" % s])
            return s

        def mm_group(bank, lhs_list, rhs_list, n, m=128, reads=(), col0=0):
            def fn(e):
                ins = None
                L = len(lhs_list)
                for i in range(L):
                    ins = e.matmul(ps[bank][0:m, col0:col0 + n], lhsT=lhs_list[i], rhs=rhs_list[i],
                                   start=(i == 0), stop=(i == L - 1))
                return ins
            return P.c("pe", fn, reads=list(reads), writes=["ps%d" % bank])

        def transposes(bank, ins_list, idt, reads, bf=True, m=128, col0=0, wkey=None):
            def fn(e):
                ins = None
                c = col0
                for a in ins_list:
                    kk = a.shape[0]
                    mm_ = a.shape[1]
                    tgt = (psb[bank] if bf else ps[bank])[0:mm_, c:c + kk]
                    ins = e.transpose(out=tgt, in_=a, identity=idt[0:kk, 0:kk])
                    c += kk
                return ins
            return P.c("pe", fn, reads=list(reads), writes=["ps%d" % bank])

        try:
            ld("sp", identf[:], c_ident, ["identf"]); ld("pool", identb[:], c_ident, ["identb"])
            ld("sp", uf[:], c_uf, ["uf"]); ld("sp", ub[:], c_ub, ["ub"]); ld("sp", of_[:], c_of, ["of"]); ld("sp", ob[:], c_ob, ["ob"])
            ld("pool", band[:], c_band, ["band"]); ld("pool", bandb[:], c_bandb, ["bandb"])
            ld("sp", rowm[:], c_rowm, ["rowm"]); ld("pool", rowmb[:], c_rowm, ["rowmb"]); ld("sp", sel8[:], c_sel8, ["sel8"])
            ld("sp", selc[:], c_selc, ["selc"])
            ld("sp", psbc, pscale.partition_broadcast(128), ["psbc", "f8_0"])
            ld("sp", bifbc[:], bif.partition_broadcast(128), ["bifbc"])
            ld("pool", wpg[:], wpg_d.rearrange("g (ci p) d -> p (g ci) d", p=128), ["wpg"])
            ld("pool", wg[:], w_in[:, OFF["ig"]:OFF["ig"] + 8].rearrange("(kc p) n -> p kc n", p=128), ["wg"])
            P.c("dve", lambda e: e.memset(onesb[:], 1.0), writes=["onesb"])
            P.c("dve", lambda e: e.tensor_tensor(
                out=wpg[:].rearrange("p (g ci) d -> p g ci d", ci=2), in0=wpg[:].rearrange("p (g ci) d -> p g ci d", ci=2),
                in1=psbc.rearrange("p (g d) -> p g d", g=4).unsqueeze(2).broadcast_to([128, 4, 2, 256]), op=ALU.mult),
                reads=["wpg", "psbc"], writes=["wpg", "f8_0"])

            def norm_tile(src_rows, gkey, dst_fn):
                fi = alloc_f(); bi = alloc_b(); ji = alloc_b(); si = rr("t", 3)
                xt = f8[fi]; hn = b4[bi]; junk = b4[ji]
                fk, bk, jk, sk = "f8_%d" % fi, "b4_%d" % bi, "b4_%d" % ji, "st%d" % si
                ld("sp", xt[:], src_rows, [fk])
                P.c("act", lambda e: e.activation(out=junk[:], in_=xt[:], func=AF.Square, accum_out=st[:, si, 0:1]),
                    reads=[fk], writes=[jk, sk])
                P.c("act", lambda e: e.activation(out=st[:, si, 1:2], in_=st[:, si, 0:1], func=AF.Sqrt, scale=1.0 / D, bias=EPS),
                    reads=[sk], writes=[sk])
                P.c("dve", lambda e: e.reciprocal(out=st[:, si, 2:3], in_=st[:, si, 1:2]), reads=[sk], writes=[sk])
                P.c("dve", lambda e: e.scalar_tensor_tensor(out=hn[:], in0=xt[:], scalar=st[:, si, 2:3], in1=gbc[:],
                                                            op0=ALU.mult, op1=ALU.mult), reads=[fk, sk, gkey], writes=[bk])
                for half in range(2):
                    bank = 4 + rr("q", 2)
                    transposes(bank, [hn[:, (half * 8 + k) * 128:(half * 8 + k + 1) * 128] for k in range(8)], identb,
                               reads=[bk, "identb"])
                    dst, dkeys = dst_fn(half)
                    eng = "act" if half == 0 else "dve"
                    if eng == "act":
                        P.c("act", lambda e, d_=dst, b_=bank: e.copy(out=d_, in_=psb[b_][:, 0:1024].rearrange("p (k t) -> p k t", k=8)),
                            reads=["ps%d" % bank], writes=dkeys)
                    else:
                        P.c("dve", lambda e, d_=dst, b_=bank: e.tensor_copy(out=d_, in_=psb[b_][:, 0:1024].rearrange("p (k t) -> p k t", k=8)),
                            reads=["ps%d" % bank], writes=dkeys)

            ld("sp", gbc[:], g_pre.partition_broadcast(128), ["gbc_pre"])
            kv_slots = [load_w(w_kv, 0, KC, cb_ * 512, 512) for cb_ in range(2)]
            for i in range(NT):
                norm_tile(xm[i * 128:(i + 1) * 128, :], "gbc_pre",
                          lambda half, i=i: (hT[:, half * 8:(half + 1) * 8, i * 128:(i + 1) * 128], [("hT", i, half)]))
            hTk = lambda i: [("hT", i, 0), ("hT", i, 1)]
            def norm_prefix(i):
                bi = alloc_b()
                hp = b4[bi]
                norm_tile(xp[i * 128:(i + 1) * 128, :], "gbc_pre",
                          lambda half, hp=hp, bi=bi: (hp[:, half * 1024:(half + 1) * 1024].rearrange("p (k t) -> p k t", k=8),
                                                      [("b4h", bi, half)]))
                P.d("sp", lambda e, hp=hp, i=i: e.dma_start(out=hps[i], in_=hp[:]), reads=[("b4h", bi, 0), ("b4h", bi, 1)],
                    writes=[("hps", i), "b4_%d" % bi])
            norm_prefix(7)
            pend_norm = list(range(7))

            def load_hp(i):
                bi = alloc_b()
                P.d("sp", lambda e, bi=bi, i=i: e.dma_start(out=b4[bi][:], in_=hps[i]), reads=[("hps", i)], writes=["b4_%d" % bi])
                return bi

            _ck("A")
            P.d("sp", lambda e: e.dma_start(out=gbc[:], in_=g_mem.partition_broadcast(128)), writes=["gbc_pre", "gbc_mem"])
            for i in range(2):
                norm_tile(memx[i * 128:(i + 1) * 128, :], "gbc_mem",
                          lambda half, i=i: (memhT[:, half * 8:(half + 1) * 8, i * 128:(i + 1) * 128], [("memhT", i, half)]))
            mhk = [("memhT", i, h) for i in range(2) for h in range(2)]
            for cb in range(4):
                s = kv_slots[cb] if cb < 2 else load_w(w_kv, 0, KC, cb * 512, 512)
                for i in range(2):
                    bank = rr("p", 4)
                    mm_group(bank, [memhT[:, k, i * 128:(i + 1) * 128] for k in range(KC)], [W[s][:, k, :] for k in range(KC)], 512,
                             reads=mhk + ["W%d" % s])
                    fi = alloc_f()
                    P.c("act", lambda e, fi=fi, bank=bank: e.copy(out=f8[fi][:, 0:512], in_=ps[bank][:]),
                        reads=["ps%d" % bank], writes=["f8_%d" % fi])
                    dst = (mk if cb < 2 else mv)[i * 128:(i + 1) * 128, (cb % 2) * 512:(cb % 2) * 512 + 512]
                    outs.append(P.d("sp", lambda e, fi=fi, dst=dst: e.dma_start(out=dst, in_=f8[fi][:, 0:512]), reads=["f8_%d" % fi]))
                    if cb >= 2:
                        P.c("dve", lambda e, bank=bank, i=i, cb=cb: e.tensor_copy(out=Vp[:, i, (cb - 2) * 512:(cb - 1) * 512], in_=ps[bank][:]),
                            reads=["ps%d" % bank], writes=[("Vp", i, cb)])
                if cb < 2:
                    for dt_ in range(4):
                        bank = rr("p", 4)
                        mm_group(bank, [W[s][:, k, dt_ * 128:(dt_ + 1) * 128] for k in range(KC)], [memhT[:, k, :] for k in range(KC)], 256,
                                 reads=mhk + ["W%d" % s])
                        P.c("dve", lambda e, bank=bank, dc=cb * 4 + dt_: e.tensor_copy(out=KTp[:, dc, :], in_=ps[bank][:, 0:256]),
                            reads=["ps%d" % bank], writes=[("KTp", cb * 4 + dt_)])
            P.v(mhk + [("R3", 0, i_) for i_ in range(8)])
            KTpk = [("KTp", i) for i in range(8)]
            Vpk = [("Vp", i, cb) for i in range(2) for cb in (2, 3)]

            P.d("sp", lambda e: e.dma_start(out=gbc[:], in_=g_pre.partition_broadcast(128)), writes=["gbc_pre", "gbc_mem"])
            _ck("B")
            _ck("C")
            def proj_block(c0, slot, evac, tiles=range(NT), ncols=512, pre=None):
                s = load_w(w_in, 0, KC, c0, ncols)
                if pre is not None:
                    pre(s)
                for i in tiles:
                    bank = rr("p", 4)
                    mm_group(bank, [hT[:, k, i * 128:(i + 1) * 128] for k in range(KC)], [W[s][:, k, 0:ncols] for k in range(KC)], ncols,
                             reads=hTk(i) + ["W%d" % s])
                    evac(i, bank, slot)

            def r3k(slot, i):
                return ("R3", slot, i)

            def ev_copy(eng="act", scale=None):
                def f(i, bank, slot):
                    if eng == "act":
                        if scale is None:
                            P.c("act", lambda e: e.copy(out=R3[:, slot, i, :], in_=ps[bank][:]), reads=["ps%d" % bank], writes=[r3k(slot, i)])
                        else:
                            P.c("act", lambda e: e.mul(out=R3[:, slot, i, :], in_=ps[bank][:], mul=scale), reads=["ps%d" % bank], writes=[r3k(slot, i)])
                    else:
                        P.c("dve", lambda e: e.tensor_copy(out=R3[:, slot, i, :], in_=ps[bank][:]), reads=["ps%d" % bank], writes=[r3k(slot, i)])
                return f

            def ev_act(func):
                def f(i, bank, slot):
                    P.c("act", lambda e: e.activation(out=R3[:, slot, i, :], in_=ps[bank][:], func=func), reads=["ps%d" % bank], writes=[r3k(slot, i)])
                return f

            def to_featmajor(src_aps, src_keys, nk, ydst_kc0, i, extra_reads=()):
                bi = alloc_b()
                for g0 in range(0, nk, 8):
                    n = min(8, nk - g0)
                    bank = 4 + rr("q", 2)
                    transposes(bank, src_aps[g0:g0 + n], identb, reads=list(src_keys) + ["identb"] + list(extra_reads))
                    P.c("act", lambda e, bank=bank, n=n, g0=g0: e.copy(out=b4[bi][:, g0 * 128:(g0 + n) * 128], in_=psb[bank][:, 0:n * 128]),
                        reads=["ps%d" % bank], writes=[("b4p", bi, g0)])
                P.d("sp", lambda e: e.dma_start(out=yin[i][:, ydst_kc0 * 128:(ydst_kc0 + nk) * 128], in_=b4[bi][:, 0:nk * 128]),
                    reads=[("b4p", bi, g0) for g0 in range(0, nk, 8)], writes=[("yin", i, ydst_kc0), "b4_%d" % bi])

            pvprev = ARENA[:, 6144:7168]
            def pv_pre(cbk):
                def f(s_):
                    bi7 = load_hp(7)
                    bank = rr("p", 4)
                    mm_group(bank, [b4[bi7][:, k * 128:(k + 1) * 128] for k in range(KC)], [W[s_][:, k, :] for k in range(KC)], 512,
                             reads=["b4_%d" % bi7, "W%d" % s_])
                    P.c("act", lambda e: e.copy(out=pvprev[:, cbk * 512:(cbk + 1) * 512], in_=ps[bank][:]),
                        reads=["ps%d" % bank], writes=[("pvprev", cbk)])
                return f

            tickE = [0]

            def hookE():
                tickE[0] += 1
                if tickE[0] % 4 == 0 and pend_norm:
                    norm_prefix(pend_norm.pop(0))

            def wrapE(ev):
                def f(i, bank, slot):
                    ev(i, bank, slot)
                    hookE()
                return f

            def ev_pv(i, bank, slot):
                P.c("act", lambda e: e.copy(out=R3[:, slot, i, :], in_=ps[bank][:]), reads=["ps%d" % bank], writes=[r3k(slot, i)])
                if i >= 7:
                    fi = alloc_f()
                    fk = "f8_%d" % fi
                    P.c("dve", lambda e: e.tensor_copy(out=f8[fi][:, 0:512], in_=ps[bank][:]), reads=["ps%d" % bank], writes=[fk])
                    if i == 7:
                        outs.append(P.d("sp", lambda e: e.dma_start(out=poolp[:, slot * 512:(slot + 1) * 512], in_=f8[fi][113:128, 0:512]),
                                        reads=[fk]))
                    else:
                        for j in range(16):
                            outs.append(P.d("sp", lambda e, j=j: e.dma_start(out=pools[j, 7:15, slot * 512:(slot + 1) * 512],
                                                                             in_=f8[fi][j * 8:(j + 1) * 8, 0:512]), reads=[fk]))
            proj_block(OFF["pv"], 0, wrapE(ev_pv), pre=pv_pre(0))
            proj_block(OFF["pv"] + 512, 1, wrapE(ev_pv), pre=pv_pre(1))
            proj_block(OFF["pz"], 2, wrapE(ev_act(AF.Silu)))
            proj_block(OFF["pz"] + 512, 3, wrapE(ev_act(AF.Silu)))
            while pend_norm:
                norm_prefix(pend_norm.pop(0))
            outs.append(P.d("sp", lambda e: e.dma_start(out=pools[:, 0:7, :], in_=spool[:, 8:15, :])))
            bufb = ARENA[0:120, 4096:6144].rearrange("p (a b) -> p a b", a=2)
            for c2 in range(2):
                ld("pool", bufb[:, c2, :], spool[c2 * 8:(c2 + 1) * 8].rearrange("j r c -> (j r) c"), [("bufb", c2)])
            for i in range(NT):
                for hb_ in range(2):
                    bank = 6 + hb_

                    def fn(e, i=i, hb_=hb_, bank=bank):
                        ins = None
                        for q4 in range(4):
                            cc = hb_ * 4 + q4
                            g = cc // 2
                            slot, c0 = cc // 4, (cc % 4) * 128
                            out = ps[bank][:, q4 * 128:(q4 + 1) * 128]
                            if i < 8:
                                kind = 0 if i == 0 else 1
                                e.matmul(out, lhsT=R3[:, slot, i, c0:c0 + 128], rhs=band[:, kind * 4 + g, :], start=True, stop=False)
                                prev = pvprev[:, cc * 128:(cc + 1) * 128] if i == 0 else R3[:, slot, i - 1, c0:c0 + 128]
                                ins = e.matmul(out, lhsT=prev, rhs=band[:, 2 * 4 + g, :], start=False, stop=True)
                            else:
                                e.matmul(out, lhsT=R3[:, slot, i, c0:c0 + 128], rhs=band[:, 3 * 4 + g, :], start=True, stop=False)
                                e.matmul(out, lhsT=bufb[:, 0, cc * 128:(cc + 1) * 128], rhs=bandb[:, g * 2 + 0, :], start=False, stop=False)
                                ins = e.matmul(out, lhsT=bufb[:, 1, cc * 128:(cc + 1) * 128], rhs=bandb[:, g * 2 + 1, :], start=False, stop=True)
                        return ins
                    rds = [r3k(0, i), r3k(1, i), "band", "bandb", ("bufb", 0), ("bufb", 1), ("pvprev", 0), ("pvprev", 1)]
                    if 0 < i < 8:
                        rds += [r3k(0, i - 1), r3k(1, i - 1)]
                    P.c("pe", fn, reads=rds, writes=["ps%d" % bank])
                bi = alloc_b()
                pT = b4[bi]
                P.c("act", lambda e, pT=pT: e.copy(out=pT[:, 0:512], in_=ps[6][:]), reads=["ps6"], writes=[("b4p", bi, 0)])
                P.c("dve", lambda e, pT=pT: e.tensor_copy(out=pT[:, 512:1024], in_=ps[7][:]), reads=["ps7"], writes=[("b4p", bi, 8)])
                yb_i = alloc_b()
                yat = b4[yb_i]
                for hb_ in range(2):
                    bank = rr("p", 4)

                    def fn2(e, hb_=hb_, bank=bank, pT=pT):
                        ins = None
                        for gg in range(2):
                            g = hb_ * 2 + gg
                            for ci in range(2):
                                ins = e.matmul(ps[bank][:, gg * 256:(gg + 1) * 256], lhsT=pT[:, (g * 2 + ci) * 128:(g * 2 + ci + 1) * 128],
                                               rhs=wpg[:, g * 2 + ci, :], start=(ci == 0), stop=(ci == 1))
                        return ins
                    P.c("pe", fn2, reads=[("b4p", bi, 0), ("b4p", bi, 8), "wpg"], writes=["ps%d" % bank])
                    P.c("dve", lambda e, bank=bank, hb_=hb_, yat=yat, i=i: e.tensor_tensor(
                        out=yat[:, hb_ * 512:(hb_ + 1) * 512], in0=ps[bank][:], in1=R3[:, 2 + hb_, i, :], op=ALU.mult),
                        reads=["ps%d" % bank, r3k(2 + hb_, i)], writes=[("b4q", yb_i, hb_)])
                P.v(["b4_%d" % bi] + [("b4p", bi, 0), ("b4p", bi, 8)])
                to_featmajor([yat[:, k * 128:(k + 1) * 128] for k in range(8)], [("b4q", yb_i, 0), ("b4q", yb_i, 1)], 8, 0, i)
                P.v(["b4_%d" % yb_i] + [("b4q", yb_i, 0), ("b4q", yb_i, 1)])

            _ck("E")
            proj_block(OFF["cq"], 0, ev_copy("act"))
            proj_block(OFF["cq"] + 512, 1, ev_copy("dve"))
            proj_block(OFF["cz"], 2, ev_act(AF.Silu))
            proj_block(OFF["cz"] + 512, 3, ev_act(AF.Silu))
            P.v([("KTs", 0), ("KTs", 1), "cqT", ("bufb", 0), ("bufb", 1), ("pvprev", 0), ("pvprev", 1)])
            KTs = ARENA[:, 4096:6144].rearrange("p (a b) -> p a b", a=8)
            cqT = ARENA[:, 6144:7168]
            ot = ARENA[:, 7168:8192]
            mx4 = sb("mx4", [128, 3, 8], F32)
            for i in range(NT):
                bank = 4 + rr("q", 2)
                transposes(bank, [R3[:, k // 4, i, (k % 4) * 128:(k % 4 + 1) * 128] for k in range(8)], identb,
                           reads=[r3k(0, i), r3k(1, i), "identb"])
                P.c("act", lambda e: e.copy(out=cqT[:, 0:1024], in_=psb[bank][:, 0:1024]), reads=["ps%d" % bank], writes=["cqT"])
                nsrc = 1 if i < 8 else 16

                def issue_k(j_):
                    bk__ = alloc_b()
                    ld("pool", b4[bk__][:].rearrange("p (m d) -> p m d", m=2), ck[j_].rearrange("(m p) d -> p m d", p=128), ["b4_%d" % bk__])
                    return bk__
                if i == 8:
                    nxt_k = issue_k(0)
                for j in range(nsrc):
                    if i == 8:
                        bk_ = nxt_k
                        bv_ = alloc_b()
                        ld("pool", b4[bv_][:].rearrange("p (m d) -> p m d", m=2), cv[j].rearrange("(m p) d -> p m d", p=128), ["b4_%d" % bv_])
                        Kb = b4[bk_]; Vb = b4[bv_]
                        for half in range(2):
                            bank = 4 + rr("q", 2)
                            transposes(bank, [Kb[:, m * 1024 + (half * 4 + dq) * 128: m * 1024 + (half * 4 + dq + 1) * 128]
                                              for dq in range(4) for m in range(2)], identb, reads=["b4_%d" % bk_, "identb"])
                            if half == 0:
                                P.c("act", lambda e: e.copy(out=KTs[:, 0:4, :], in_=psb[bank][:, 0:1024].rearrange("p (c m) -> p c m", c=4)),
                                    reads=["ps%d" % bank], writes=[("KTs", 0)])
                            else:
                                P.c("dve", lambda e: e.tensor_copy(out=KTs[:, 4:8, :], in_=psb[bank][:, 0:1024].rearrange("p (c m) -> p c m", c=4)),
                                    reads=["ps%d" % bank], writes=[("KTs", 1)])
                        kvk = [("KTs", 0), ("KTs", 1)]
                        vk = ["b4_%d" % bv_]
                        kt_of = lambda h, dc: KTs[:, h * 2 + dc, :]
                        v_of = lambda h, m, Vb=Vb: Vb[:, m * 1024 + h * 256: m * 1024 + (h + 1) * 256]
                    else:
                        kvk = KTpk
                        vk = Vpk
                        kt_of = lambda h, dc: KTp[:, h * 2 + dc, :]
                        v_of = lambda h, m: Vp[:, m, h * 256:(h + 1) * 256]
                    si = rr("t", 3)
                    mk_ = "mx%d" % si
                    fs = alloc_f()
                    sc = f8[fs]
                    sbanks = []
                    for hb_ in range(2):
                        bank = rr("p", 4)
                        sbanks.append(bank)

                        def fsc(e, hb_=hb_, bank=bank, kt_of=kt_of):
                            ins = None
                            for hh in range(2):
                                h = hb_ * 2 + hh
                                for dc in range(2):
                                    ins = e.matmul(ps[bank][:, hh * 256:(hh + 1) * 256], lhsT=cqT[:, (h * 2 + dc) * 128:(h * 2 + dc + 1) * 128],
                                                   rhs=kt_of(h, dc), start=(dc == 0), stop=(dc == 1))
                            return ins
                        P.c("pe", fsc, reads=["cqT"] + kvk, writes=["ps%d" % bank])
                        P.c("dve", lambda e: e.tensor_reduce(out=mx4[:, si, hb_ * 2:hb_ * 2 + 2], in_=ps[bank][:].rearrange("p (h m) -> p h m", h=2),
                                                             axis=AX.X, op=ALU.max), reads=["ps%d" % bank], writes=[(mk_, hb_)])
                        P.c("dve", lambda e: e.tensor_tensor(out=sc[:, hb_ * 512:(hb_ + 1) * 512].rearrange("p (h m) -> p h m", h=2),
                                                             in0=ps[bank][:].rearrange("p (h m) -> p h m", h=2),
                                                             in1=mx4[:, si, hb_ * 2:hb_ * 2 + 2].unsqueeze(2).broadcast_to([128, 2, 256]), op=ALU.subtract),
                            reads=["ps%d" % bank, (mk_, hb_)], writes=[("f8c", fs, hb_)])
                    ba = alloc_b()
                    ea = b4[ba]
                    P.c("act", lambda e: e.activation(out=ea[:, 0:1024], in_=sc[:, 0:1024], func=AF.Exp, scale=1.0 / 16.0),
                        reads=[("f8c", fs, 0), ("f8c", fs, 1)], writes=["b4_%d" % ba])
                    P.c("dve", lambda e: e.tensor_reduce(out=mx4[:, si, 4:8], in_=ea[:, 0:1024].rearrange("p (h m) -> p h m", h=4), axis=AX.X, op=ALU.add),
                        reads=["b4_%d" % ba], writes=[(mk_, 2)])
                    P.c("dve", lambda e: e.reciprocal(out=mx4[:, si, 4:8], in_=mx4[:, si, 4:8]), reads=[(mk_, 2)], writes=[(mk_, 2)])
                    if i == 8:
                        P.c("dve", lambda e: e.tensor_scalar(out=mx4[:, si, 4:8], in0=mx4[:, si, 4:8], scalar1=rowm[:, j:j + 1], scalar2=None, op0=ALU.mult),
                            reads=[(mk_, 2), "rowm"], writes=[(mk_, 2)])
                    P.c("dve", lambda e: e.tensor_tensor(out=ea[:, 1024:2048].rearrange("p (h m) -> p h m", h=4),
                                                         in0=ea[:, 0:1024].rearrange("p (h m) -> p h m", h=4),
                                                         in1=mx4[:, si, 4:8].unsqueeze(2).broadcast_to([128, 4, 256]), op=ALU.mult),
                        reads=["b4_%d" % ba, (mk_, 2)], writes=[("b4a", ba)])
                    bank = 4 + rr("q", 2)
                    transposes(bank, [ea[:, 1024 + k * 128:1024 + (k + 1) * 128] for k in range(8)], identb, reads=[("b4a", ba), "identb"])
                    bt_ = alloc_b()
                    aTt = b4[bt_]
                    P.c("act", lambda e: e.copy(out=aTt[:, 0:1024], in_=psb[bank][:, 0:1024]), reads=["ps%d" % bank], writes=["b4_%d" % bt_])
                    if i == 8 and j + 1 < nsrc:
                        nxt_k = issue_k(j + 1)
                    for hb_ in range(2):
                        def fo(e, hb_=hb_, v_of=v_of, aTt=aTt, j=j, nsrc=nsrc):
                            ins = None
                            for hh in range(2):
                                h = hb_ * 2 + hh
                                for m in range(2):
                                    ins = e.matmul(ps[6 + hb_][:, hh * 256:(hh + 1) * 256], lhsT=aTt[:, (h * 2 + m) * 128:(h * 2 + m + 1) * 128],
                                                   rhs=v_of(h, m), start=(j == 0 and hh == 0 and m == 0),
                                                   stop=(j == nsrc - 1 and hh == 1 and m == 1), skip_group_check=True)
                            return ins
                        P.c("pe", fo, reads=["b4_%d" % bt_] + vk, writes=["ps%d" % (6 + hb_)])
                for hb_ in range(2):
                    P.c("dve", lambda e: e.tensor_tensor(out=ot[:, hb_ * 512:(hb_ + 1) * 512], in0=ps[6 + hb_][:], in1=R3[:, 2 + hb_, i, :], op=ALU.mult),
                        reads=["ps%d" % (6 + hb_), r3k(2 + hb_, i)], writes=[("ot", hb_)])
                to_featmajor([ot[:, k * 128:(k + 1) * 128] for k in range(8)], [("ot", 0), ("ot", 1)], 8, 24, i)

            for ch in range(NCH):
                if ch < 8:
                    bi = load_hp(ch)
                    lhs = [b4[bi][:, k * 128:(k + 1) * 128] for k in range(KC)]
                    rds = ["b4_%d" % bi, "wg"]
                else:
                    i = ch - 8
                    lhs = [hT[:, k, i * 128:(i + 1) * 128] for k in range(KC)]
                    rds = hTk(i) + ["wg"]
                bank = rr("p", 4)
                mm_group(bank, lhs, [wg[:, k, :] for k in range(KC)], 8, reads=rds)
                P.c("dve", lambda e, bank=bank, ch=ch: e.tensor_tensor(out=G[:, ch, :], in0=ps[bank][:, 0:8], in1=bifbc[:], op=ALU.add),
                    reads=["ps%d" % bank, "bifbc"], writes=["G"])
            ig = G[:, :, 0:4]; fg = G[:, :, 4:8]
            P.c("act", lambda e: e.activation(out=t1[:], in_=fg, func=AF.Abs), reads=["G"], writes=["t1"])
            P.c("act", lambda e: e.activation(out=t1[:], in_=t1[:], func=AF.Exp, scale=-1.0), reads=["t1"], writes=["t1"])
            P.c("act", lambda e: e.activation(out=t1[:], in_=t1[:], func=AF.Ln, bias=1.0), reads=["t1"], writes=["t1"])
            P.c("dve", lambda e: e.scalar_tensor_tensor(out=lf[:], in0=fg, scalar=0.0, in1=t1[:], op0=ALU.min, op1=ALU.subtract),
                reads=["G", "t1"], writes=["lf"])
            lff = lf[:].rearrange("p c h -> p (c h)")

            def cums(e):
                e.matmul(ps[6][:, 0:64], lhsT=uf[:], rhs=lff[:, 0:64], start=True, stop=True)
                e.matmul(ps[6][:, 64:68], lhsT=ub[:], rhs=lff[:, 64:68], start=True, stop=True)
                e.matmul(ps[6][:, 128:192], lhsT=of_[:], rhs=lff[:, 0:64], start=True, stop=True)
                return e.matmul(ps[6][:, 192:196], lhsT=ob[:], rhs=lff[:, 64:68], start=True, stop=True)
            P.c("pe", cums, reads=["lf", "uf", "ub", "of", "ob"], writes=["ps6"])
            P.c("dve", lambda e: e.tensor_copy(out=ball[:].rearrange("p c h -> p (c h)"), in_=ps[6][:, 0:68]), reads=["ps6"], writes=["ball"])
            P.c("dve", lambda e: e.tensor_copy(out=blast[:].rearrange("p c h -> p (c h)"), in_=ps[6][:, 128:196]), reads=["ps6"], writes=["blast"])
            P.c("dve", lambda e: e.tensor_tensor(out=aall[:], in0=ig, in1=ball[:], op=ALU.subtract), reads=["G", "ball"], writes=["aall"])
            P.c("pe", lambda e: e.transpose(out=ps[7][0:68, 0:128], in_=aall[:].rearrange("p c h -> p (c h)"), identity=identf[:]),
                reads=["aall", "identf"], writes=["ps7"])
            P.c("dve", lambda e: e.tensor_reduce(out=amx[0:64, 0:1], in_=ps[7][0:64, 0:128], axis=AX.X, op=ALU.max), reads=["ps7"], writes=["amx"])
            P.c("dve", lambda e: e.tensor_reduce(out=amx[64:68, 0:16], in_=ps[7][64:68, 0:128].rearrange("p (j i) -> p j i", i=8),
                                                 axis=AX.X, op=ALU.max), reads=["ps7"], writes=["amx2"])
            P.c("dve", lambda e: e.tensor_copy(out=aT[0:64, :], in_=amx[0:64, 0:1].broadcast_to([64, 128])), reads=["amx"], writes=["aT"])
            P.c("dve", lambda e: e.tensor_copy(out=aT[64:68, :].rearrange("p (j i) -> p j i", i=8),
                                               in_=amx[64:68, 0:16].unsqueeze(2).broadcast_to([4, 16, 8])), reads=["amx2"], writes=["aT2"])
            P.c("pe", lambda e: e.transpose(out=ps[7][:, 128:196], in_=aT[:], identity=identf[0:68, 0:68]),
                reads=["aT", "aT2", "identf"], writes=["ps7"])
            P.c("dve", lambda e: e.tensor_copy(out=amaxbc[:].rearrange("p c h -> p (c h)"), in_=ps[7][:, 128:196]), reads=["ps7"], writes=["amaxbc"])
            P.c("dve", lambda e: e.memset(m0[:], 0.0), writes=["m0"])
            ld("sp", m0[:, 16, :], bass.AP(sm.tensor, 0, [[4, 16], [0, 8], [1, 4]]), ["m0s"], reads=["m0"])
            for ch in range(16):
                P.c("dve", lambda e, ch=ch: e.tensor_tensor(out=Mp[:, ch, :], in0=amaxbc[:, ch, :], in1=m0[:, ch, :], op=ALU.max),
                    reads=["amaxbc", "m0"], writes=["Mp"])
                nxt = ch + 1 if ch < 15 else 17
                P.c("dve", lambda e, ch=ch, nxt=nxt: e.tensor_tensor(out=m0[:, nxt, :], in0=blast[:, ch, :], in1=Mp[:, ch, :], op=ALU.add),
                    reads=["blast", "Mp"], writes=["m0"])
                if ch == 7:
                    P.c("dve", lambda e: e.tensor_scalar(out=m0[:, 8, :], in0=m0[:, 8, :], scalar1=selc[:, 0:1], scalar2=None, op0=ALU.mult),
                        reads=["m0", "selc"], writes=["m0"])
            P.c("dve", lambda e: e.tensor_tensor(out=Mp[:, 16, :], in0=amaxbc[:, 16, :], in1=m0[:, 16, :], op=ALU.max),
                reads=["amaxbc", "m0", "m0s"], writes=["Mp"])
            P.c("dve", lambda e: e.tensor_tensor(out=small[:, 0:4], in0=blast[:, 16, :], in1=Mp[:, 16, :], op=ALU.add),
                reads=["blast", "Mp"], writes=["small"])
            outs.append(P.d("sp", lambda e: e.dma_start(out=mp, in_=m0[0:1, 17, :]), reads=["m0"]))
            outs.append(P.d("sp", lambda e: e.dma_start(out=ms, in_=bass.AP(small, 0, [[16 * 8, 16], [1, 4]])), reads=["small"]))
            P.c("dve", lambda e: e.tensor_tensor(out=wall[:], in0=aall[:], in1=Mp[:], op=ALU.subtract), reads=["aall", "Mp"], writes=["wall"])
            P.c("act", lambda e: e.activation(out=wall[:], in_=wall[:], func=AF.Exp), reads=["wall"], writes=["wall"])
            P.c("dve", lambda e: e.tensor_tensor(out=dall[:, 0:NCH, :], in0=m0[:, 0:NCH, :], in1=Mp[:], op=ALU.subtract),
                reads=["m0", "m0s", "Mp"], writes=["dall"])
            P.c("act", lambda e: e.activation(out=dall[:, 0:NCH, :], in_=dall[:, 0:NCH, :], func=AF.Exp), reads=["dall"], writes=["dall"])
            P.c("dve", lambda e: e.memset(dall[:, NCH, :], 1.0), reads=["dall"], writes=["dall"])
            P.c("dve", lambda e: e.tensor_tensor(out=thr[:], in0=ball[:], in1=Mp[:], op=ALU.add), reads=["ball", "Mp"], writes=["thr"])
            P.c("act", lambda e: e.activation(out=thr[:], in_=thr[:], func=AF.Exp, scale=-1.0), reads=["thr"], writes=["thr"])
            P.c("dve", lambda e: e.memset(tmpg[:, 7, :], 1.0), writes=["tmpg"])
            for c_ in range(6, -1, -1):
                P.c("dve", lambda e: e.tensor_tensor(out=tmpg[:, c_, :], in0=tmpg[:, c_ + 1, :], in1=dall[:, c_ + 1, :], op=ALU.mult),
                    reads=["tmpg", "dall"], writes=["tmpg"])
            P.c("dve", lambda e: e.tensor_tensor(out=wall[:, 0:8, :], in0=wall[:, 0:8, :], in1=tmpg[:, 0:8, :], op=ALU.mult),
                reads=["tmpg", "wall"], writes=["wall"])
            P.c("dve", lambda e: e.tensor_tensor(out=dsrc[:], in0=dall[:, 16, :].unsqueeze(1).broadcast_to([128, 16, 4]),
                                                 in1=sel8[:].unsqueeze(2).broadcast_to([128, 16, 4]), op=ALU.mult),
                reads=["dall", "sel8"], writes=["dsrc"])
            P.c("pe", lambda e: e.matmul(ps[6][:, 256:320], lhsT=of_[:], rhs=dsrc[:].rearrange("p j h -> p (j h)"), start=True, stop=True),
                reads=["dsrc", "of"], writes=["ps6"])
            P.c("dve", lambda e: e.tensor_copy(out=dsall[:].rearrange("p j h -> p (j h)"), in_=ps[6][:, 256:320]), reads=["ps6"], writes=["dsall"])
            P.c("pe", lambda e: e.matmul(ps[6][0:16, 320:324], lhsT=sel8[:], rhs=dall[:, 16, :], start=True, stop=True),
                reads=["dall", "sel8", "dsall"], writes=["ps6"])
            P.c("dve", lambda e: e.tensor_copy(out=d16[:], in_=ps[6][0:16, 320:324]), reads=["ps6"], writes=["d16"])
            GK = ["wall", "dall", "thr", "dsall", "d16"]

            _ck("G")
            SC = 512.0 ** -0.5
            ld("pool", band[:], c_blk, ["band", "blk"])
            kp = sb("kp", [128, 512], BF16)
            PT = sb("PT", [128, 128], BF16)
            qk = sb("qkT", [128, 1024], BF16)
            rr_ = sb("rr_", [128, 4], F32)
            qk_s = sb("qk_s", [128, 1024], BF16)
            PT_s = sb("PT_s", [128, 128], BF16)
            kp_s = sb("kp_s", [128, 512], BF16)
            rr_s = sb("rr_s", [128, 4], F32)
            pending = []
            posts = {}

            def make_sample(h):
                si_ = 8 + (h % 2)
                ch = 16
                stt = {}

                def issue_c0(j_):
                    fi_ = alloc_f()
                    ld("sp", f8[fi_][:].rearrange("p (vc k) -> p vc k", vc=4), sC[j_, h].rearrange("(vc p) k -> p vc k", p=128), ["f8_%d" % fi_])
                    return fi_

                def pre():
                    bank = 4 + rr("q", 2)
                    transposes(bank, [R3[:, 0, si_, c * 128:(c + 1) * 128] for c in range(4)] + [R3[:, 1, si_, c * 128:(c + 1) * 128] for c in range(4)],
                               identb, reads=[r3k(0, si_), r3k(1, si_), "identb"])
                    P.c("act", lambda e: e.copy(out=qk_s[:, 0:1024], in_=psb[bank][:, 0:1024]), reads=["ps%d" % bank], writes=["qk_s"])
                    bank = rr("p", 4)
                    mm_group(bank, [qk_s[:, 512 + c * 128:512 + (c + 1) * 128] for c in range(4)], [qk_s[:, c * 128:(c + 1) * 128] for c in range(4)], 128,
                             reads=["qk_s"])
                    P.c("dve", lambda e: e.scalar_tensor_tensor(out=PT_s[:], in0=ps[bank][:, 0:128], scalar=wall[:, ch, h:h + 1],
                                                                in1=ub[:], op0=ALU.mult, op1=ALU.mult),
                        reads=["ps%d" % bank, "wall", "ub"], writes=["PT_s"])
                    P.c("dve", lambda e: e.tensor_scalar(out=kp_s[:], in0=R3[:, 1, si_, :], scalar1=wall[:, ch, h:h + 1], scalar2=None, op0=ALU.mult),
                        reads=[r3k(1, si_), "wall"], writes=["kp_s"])
                    ld("sp", n0[:], sn[:, h, :], ["n0"])
                    fe = alloc_f()
                    n0e = f8[fe][:, 0:512]
                    ld("sp", n0e, bass.AP(sn.tensor, h * 512, [[2048, 16], [0, 8], [1, 512]]), ["f8_%d" % fe])
                    bj = alloc_b()
                    P.c("dve", lambda e: e.scalar_tensor_tensor(out=b4[bj][:, 0:512], in0=R3[:, 0, si_, :], scalar=dall[:, ch, h:h + 1],
                                                                in1=n0e, op0=ALU.mult, op1=ALU.mult, accum_out=rr_s[:, 2:3]),
                        reads=[r3k(0, si_), "dall", "f8_%d" % fe], writes=["b4_%d" % bj, "rrs2"])
                    stt["nxt"] = issue_c0(0)

                def unit(j):
                    fi = stt["nxt"]
                    if j + 1 < 16:
                        stt["nxt"] = issue_c0(j + 1)
                    bc_ = alloc_b(); bct = alloc_b()
                    C0f = f8[fi]; C0b = b4[bc_]; C0T = b4[bct]
                    fk = "f8_%d" % fi
                    P.c("act", lambda e: e.copy(out=C0b[:], in_=C0f[:]), reads=[fk], writes=["b4_%d" % bc_])
                    for kc in range(4):
                        bank = 4 + rr("q", 2)
                        transposes(bank, [C0b[:, vc * 512 + kc * 128: vc * 512 + (kc + 1) * 128] for vc in range(4)], identb,
                                   reads=["b4_%d" % bc_, "identb"])
                        P.c("act", lambda e: e.activation(out=C0T[:, kc * 512:(kc + 1) * 512], in_=psb[bank][:, 0:512],
                                                          func=AF.Copy, scale=dsall[:, j, h:h + 1]),
                            reads=["ps%d" % bank, "dsall"], writes=[("b4c", bct, kc)])
                    bm = alloc_b()
                    qm = b4[bm]
                    P.c("dve", lambda e: e.tensor_tensor(out=qm[:, 0:512].rearrange("p (c t) -> p c t", c=4),
                                                         in0=qk_s[:, 0:512].rearrange("p (c t) -> p c t", c=4),
                                                         in1=blk[:, j, :].unsqueeze(1).broadcast_to([128, 4, 128]), op=ALU.mult),
                        reads=["qk_s", "blk"], writes=["b4_%d" % bm])

                    def fnum(e):
                        ins = None
                        if j == 0:
                            e.matmul(ps[7][:], lhsT=PT_s[:], rhs=R3[:, 2, si_, :], start=True, stop=False)
                        for c in range(4):
                            ins = e.matmul(ps[7][:], lhsT=qm[:, c * 128:(c + 1) * 128], rhs=C0T[:, c * 512:(c + 1) * 512],
                                           start=False, stop=(j == 15 and c == 3))
                        return ins
                    P.c("pe", fnum, reads=["PT_s", r3k(2, si_), "b4_%d" % bm] + [("b4c", bct, kc) for kc in range(4)], writes=["ps7"])
                    P.c("dve", lambda e: e.tensor_tensor(out=wj[:, 0:1], in0=wall[:, ch, h:h + 1], in1=rowm[:, j:j + 1], op=ALU.mult),
                        reads=["wall", "rowm"], writes=["wj"])
                    bkj = alloc_b()
                    P.c("dve", lambda e: e.tensor_scalar(out=b4[bkj][:, 0:512], in0=R3[:, 1, si_, :], scalar1=wj[:, 0:1], scalar2=None,
                                                         op0=ALU.mult), reads=[r3k(1, si_), "wj"], writes=["b4_%d" % bkj])
                    for vc in range(4):
                        bank = rr("p", 4)
                        mm_group(bank, [R3[:, 2, si_, vc * 128:(vc + 1) * 128]], [b4[bkj][:, 0:512]], 512, reads=[r3k(2, si_), "b4_%d" % bkj])
                        P.c("dve", lambda e: e.scalar_tensor_tensor(
                            out=C0f[:, vc * 512:(vc + 1) * 512], in0=C0f[:, vc * 512:(vc + 1) * 512], scalar=dsall[:, j, h:h + 1], in1=ps[bank][:],
                            op0=ALU.mult, op1=ALU.add), reads=["ps%d" % bank, fk, "dsall", "b4_%d" % bc_], writes=[("f8c", fi, vc)])
                    outs.append(P.d("sp", lambda e: e.dma_start(out=Cs[j, h].rearrange("(vc p) k -> p vc k", p=128),
                                                               in_=C0f[:].rearrange("p (vc k) -> p vc k", vc=4)),
                                    reads=[("f8c", fi, vc) for vc in range(4)]))

                def post():
                    bank = rr("p", 4)
                    mm_group(bank, [rowmb[:]], [kp_s[:]], 512, m=16, reads=["rowmb", "kp_s"])
                    P.c("dve", lambda e: e.scalar_tensor_tensor(out=nout[:], in0=n0[:], scalar=d16[:, h:h + 1], in1=ps[bank][0:16, :],
                                                                op0=ALU.mult, op1=ALU.add), reads=["ps%d" % bank, "n0", "d16"], writes=["nout"])
                    outs.append(P.d("sp", lambda e: e.dma_start(out=ns[:, h, :], in_=nout[:]), reads=["nout"], writes=["nout_d"]))
                    P.v(["nout"] + ["nout_d"])
                    P.c("pe", lambda e: e.matmul(ps[6][:, 410:411], lhsT=PT_s[:], rhs=onesb[:], start=True, stop=True), reads=["PT_s", "onesb"], writes=["ps6"])
                    P.c("dve", lambda e: e.tensor_tensor(out=rr_s[:, 0:1], in0=ps[6][:, 410:411], in1=rr_s[:, 2:3], op=ALU.add),
                        reads=["ps6", "rrs2"], writes=["rrs"])
                    P.c("act", lambda e: e.activation(out=rr_s[:, 3:4], in_=rr_s[:, 0:1], func=AF.Abs), reads=["rrs"], writes=["rrs3"])
                    P.c("dve", lambda e: e.tensor_tensor(out=rr_s[:, 0:1], in0=rr_s[:, 3:4], in1=thr[:, ch, h:h + 1], op=ALU.max),
                        reads=["rrs3", "thr"], writes=["rrs"])
                    P.c("dve", lambda e: e.reciprocal(out=rr_s[:, 1:2], in_=rr_s[:, 0:1]), reads=["rrs"], writes=["rrs1"])
                    by = alloc_b()
                    P.c("dve", lambda e: e.scalar_tensor_tensor(out=b4[by][:, 0:512], in0=ps[7][:], scalar=rr_s[:, 1:2],
                                                                in1=R3[:, 3, si_, :], op0=ALU.mult, op1=ALU.mult),
                        reads=["ps7", "rrs1", r3k(3, si_)], writes=[("b4q", by, 0)])
                    to_featmajor([b4[by][:, c * 128:(c + 1) * 128] for c in range(4)], [("b4q", by, 0)], 4, 8 + h * 4, 8)
                return pre, unit, post

            tick = [0]

            def hook():
                tick[0] += 1
                if tick[0] % 3 == 0 and pending:
                    pending.pop(0)()

            for h in range(4):
                ri = lambda i, h=h: i if i < 8 else 8 + (h % 2)
                P.c("dve", lambda e: e.memset(Cst, 0.0), reads=["Cb"], writes=["Cst", "gbc_pre", "gbc_mem"])
                P.c("dve", lambda e: e.memset(Cb[:], 0.0), writes=["Cb"])
                P.c("dve", lambda e: e.memset(nst[:], 0.0), reads=["nb"], writes=["nst"])
                P.c("dve", lambda e: e.memset(nb[:], 0.0), writes=["nb"])
                sk_ = load_w(w_in, 0, KC, OFF["k"] + h * 512, 512)
                sv_ = load_w(w_in, 0, KC, OFF["v"] + h * 512, 512)

                def state_update(ch, kp_ap, kp_keys, v_ap, v_keys, last):
                    for kc in range(4):
                        bank = rr("p", 4)
                        mm_group(bank, [kp_ap[:, kc * 128:(kc + 1) * 128]], [v_ap], 512, reads=list(kp_keys) + list(v_keys))
                        P.c("dve", lambda e, bank=bank, kc=kc: e.scalar_tensor_tensor(
                            out=Cst[:, kc, :], in0=Cst[:, kc, :], scalar=dall[:, ch, h:h + 1], in1=ps[bank][:], op0=ALU.mult, op1=ALU.add),
                            reads=["ps%d" % bank, "Cst", "dall"], writes=["Cst"])

                    def fn(e):
                        ins = None
                        for kc in range(4):
                            ins = e.matmul(ps[6][:, 400 + kc:401 + kc], lhsT=kp_ap[:, kc * 128:(kc + 1) * 128], rhs=onesb[:], start=True, stop=True)
                        return ins
                    P.c("pe", fn, reads=list(kp_keys) + ["onesb"], writes=["ps6"])
                    P.c("dve", lambda e: e.scalar_tensor_tensor(out=nst[:], in0=nst[:], scalar=dall[:, ch, h:h + 1], in1=ps[6][:, 400:404],
                                                                op0=ALU.mult, op1=ALU.add), reads=["ps6", "nst", "dall"], writes=["nst"])
                    if not last:
                        nxt = ch + 1
                        P.c("act", lambda e: e.activation(out=Cb[:], in_=Cst[:], func=AF.Copy, scale=dall[:, nxt, h:h + 1]),
                            reads=["Cst", "dall"], writes=["Cb"])
                        P.c("act", lambda e: e.activation(out=nb[:], in_=nst[:], func=AF.Copy, scale=dall[:, nxt, h:h + 1]),
                            reads=["nst", "dall"], writes=["nb"])

                for ch in range(8):
                    bi = load_hp(ch)
                    lhs = [b4[bi][:, k * 128:(k + 1) * 128] for k in range(KC)]
                    bank = rr("p3", 3)
                    mm_group(bank, lhs, [W[sk_][:, k, :] for k in range(KC)], 512, reads=["b4_%d" % bi, "W%d" % sk_])
                    bk2 = alloc_b()
                    kpp = b4[bk2][:, 0:512]
                    P.c("dve", lambda e: e.tensor_scalar(out=kpp, in0=ps[bank][:], scalar1=wall[:, ch, h:h + 1], scalar2=SC,
                                                         op0=ALU.mult, op1=ALU.mult), reads=["ps%d" % bank, "wall"], writes=["b4_%d" % bk2])
                    bank = rr("p3", 3)
                    mm_group(bank, lhs, [W[sv_][:, k, :] for k in range(KC)], 512, reads=["b4_%d" % bi, "W%d" % sv_])
                    bv = alloc_b()
                    vpp = b4[bv][:, 0:512]
                    P.c("act", lambda e: e.copy(out=vpp, in_=ps[bank][:]), reads=["ps%d" % bank], writes=["b4_%d" % bv])

                    def facc(e, kpp=kpp, vpp=vpp, ch=ch):
                        ins = None
                        for kc in range(4):
                            e.matmul(ps[4 + kc][:], lhsT=kpp[:, kc * 128:(kc + 1) * 128], rhs=vpp, start=(ch == 0), stop=(ch == 7))
                        for kc in range(4):
                            ins = e.matmul(ps[3][:, kc:kc + 1], lhsT=kpp[:, kc * 128:(kc + 1) * 128], rhs=onesb[:],
                                           start=(ch == 0 and kc == 0), stop=(ch == 7 and kc == 3), skip_group_check=True)
                        return ins
                    P.c("pe", facc, reads=["b4_%d" % bk2, "b4_%d" % bv, "onesb"], writes=["ps3", "ps4", "ps5", "ps6", "ps7"])
                for kc in range(4):
                    if kc % 2 == 0:
                        P.c("dve", lambda e: e.tensor_copy(out=Cst[:, kc, :], in_=ps[4 + kc][:]), reads=["ps%d" % (4 + kc)], writes=["Cst"])
                    else:
                        P.c("act", lambda e: e.copy(out=Cst[:, kc, :], in_=ps[4 + kc][:]), reads=["ps%d" % (4 + kc)], writes=["Cst"])
                P.c("dve", lambda e: e.tensor_copy(out=nst[:], in_=ps[3][:, 0:4]), reads=["ps3"], writes=["nst"])
                P.c("act", lambda e: e.activation(out=Cb[:], in_=Cst[:], func=AF.Copy, scale=dall[:, 8, h:h + 1]), reads=["Cst", "dall"], writes=["Cb"])
                P.c("act", lambda e: e.activation(out=nb[:], in_=nst[:], func=AF.Copy, scale=dall[:, 8, h:h + 1]), reads=["nst", "dall"], writes=["nb"])
                _ck("F%dp" % h)
                for i in range(NT):
                    bank = rr("p", 4)
                    mm_group(bank, [hT[:, k, i * 128:(i + 1) * 128] for k in range(KC)], [W[sk_][:, k, :] for k in range(KC)], 512,
                             reads=hTk(i) + ["W%d" % sk_])
                    ev_copy("act", SC)(ri(i), bank, 1)
                    hook()
                for i in range(NT):
                    bank = rr("p", 4)
                    mm_group(bank, [hT[:, k, i * 128:(i + 1) * 128] for k in range(KC)], [W[sv_][:, k, :] for k in range(KC)], 512,
                             reads=hTk(i) + ["W%d" % sv_])
                    ev_copy("dve")(ri(i), bank, 2)
                    hook()

                def wrap(ev):
                    def f(i, bank, slot):
                        ev(ri(i), bank, slot)
                        hook()
                    return f
                proj_block(OFF["q"] + h * 512, 0, wrap(ev_copy("dve")))
                proj_block(OFF["o"] + h * 512, 3, wrap(ev_act(AF.Sigmoid)))

                def ev_z(i, bank, slot):
                    bz = alloc_b()
                    P.c("act", lambda e: e.activation(out=b4[bz][:, 0:512], in_=ps[bank][:], func=AF.Silu), reads=["ps%d" % bank], writes=["b4_%d" % bz])
                    P.c("dve", lambda e: e.tensor_tensor(out=R3[:, 3, i, :], in0=R3[:, 3, i, :], in1=b4[bz][:, 0:512], op=ALU.mult),
                        reads=["b4_%d" % bz, r3k(3, i)], writes=[r3k(3, i)])
                proj_block(OFF["z"] + h * 512, 3, wrap(ev_z))

                _ck("F%dj" % h)
                while pending:
                    pending.pop(0)()
                if h > 0:
                    posts[h - 1]()
                if h == 3:
                    pre_, unit_, post_ = make_sample(h)
                    pre_()
                    for j_ in range(16):
                        pending.append(lambda j_=j_, unit_=unit_: unit_(j_))
                for i in range(8):
                    ch = 8 + i
                    bt = 92
                    bank = 4 + rr("q", 2)
                    transposes(bank, [R3[:, 0, i, c * 128:(c + 1) * 128] for c in range(4)] + [R3[:, 1, i, c * 128:(c + 1) * 128] for c in range(4)],
                               identb, reads=[r3k(0, i), r3k(1, i), "identb"])
                    P.c("act", lambda e, bank=bank, qk=qk: e.copy(out=qk[:, 0:1024], in_=psb[bank][:, 0:1024]), reads=["ps%d" % bank], writes=["b4_%d" % bt])
                    qT = lambda c, qk=qk: qk[:, c * 128:(c + 1) * 128]
                    kT = lambda c, qk=qk: qk[:, 512 + c * 128:512 + (c + 1) * 128]
                    bank = rr("p", 4)
                    mm_group(bank, [kT(c) for c in range(4)], [qT(c) for c in range(4)], 128, reads=["b4_%d" % bt])
                    P.c("dve", lambda e: e.scalar_tensor_tensor(out=PT[:], in0=ps[bank][:, 0:128], scalar=wall[:, ch, h:h + 1],
                                                                in1=uf[:], op0=ALU.mult, op1=ALU.mult),
                        reads=["ps%d" % bank, "wall", "uf"], writes=["PT"])
                    P.c("dve", lambda e: e.tensor_scalar(out=kp[:], in0=R3[:, 1, i, :], scalar1=wall[:, ch, h:h + 1], scalar2=None, op0=ALU.mult),
                        reads=[r3k(1, i), "wall"], writes=["kp"])
                    nbank = rr("p", 4)
                    mm_group(nbank, [PT[:]] + [qT(c) for c in range(4)], [R3[:, 2, i, :]] + [Cb[:, c, :] for c in range(4)], 512,
                             reads=["PT", r3k(2, i), "b4_%d" % bt, "Cb"])

                    def fden(e, qT=qT):
                        e.matmul(ps[6][:, 410:411], lhsT=PT[:], rhs=onesb[:], start=True, stop=False)
                        ins = None
                        for c in range(4):
                            ins = e.matmul(ps[6][:, 410:411], lhsT=qT(c), rhs=nb[:, c:c + 1], start=False, stop=(c == 3))
                        return ins
                    P.c("pe", fden, reads=["PT", "onesb", "b4_%d" % bt, "nb"], writes=["ps6"])
                    P.c("act", lambda e: e.activation(out=rr_[:, 3:4], in_=ps[6][:, 410:411], func=AF.Abs), reads=["ps6"], writes=["rr3"])
                    P.c("dve", lambda e: e.tensor_tensor(out=rr_[:, 0:1], in0=rr_[:, 3:4], in1=thr[:, ch, h:h + 1], op=ALU.max),
                        reads=["rr3", "thr"], writes=["rr_"])
                    P.c("dve", lambda e: e.reciprocal(out=rr_[:, 1:2], in_=rr_[:, 0:1]), reads=["rr_"], writes=["rr1"])
                    by = alloc_b()
                    P.c("dve", lambda e: e.scalar_tensor_tensor(out=b4[by][:, 0:512], in0=ps[nbank][:], scalar=rr_[:, 1:2],
                                                                in1=R3[:, 3, i, :], op0=ALU.mult, op1=ALU.mult),
                        reads=["ps%d" % nbank, "rr1", r3k(3, i)], writes=[("b4q", by, 0)])
                    to_featmajor([b4[by][:, c * 128:(c + 1) * 128] for c in range(4)], [("b4q", by, 0)], 4, 8 + h * 4, i)
                    state_update(ch, kp, ["kp"], R3[:, 2, i, :], [r3k(2, i)], i == 7)
                    if h == 3:
                        for _ in range(2):
                            if pending:
                                pending.pop(0)()
                if h == 3:
                    while pending:
                        pending.pop(0)()
                    post_()
                _ck("F%dm" % h)
                _ck("F%ds" % h)
                for vc in range(4):
                    bank = rr("p", 4)
                    transposes(bank, [Cst[:, kc, vc * 128:(vc + 1) * 128] for kc in range(4)], identf, reads=["Cst", "identf"], bf=False)
                    fi = alloc_f()
                    P.c("act", lambda e, bank=bank, fi=fi: e.copy(out=f8[fi][:, 0:512], in_=ps[bank][:]), reads=["ps%d" % bank], writes=["f8_%d" % fi])
                    outs.append(P.d("sp", lambda e, fi=fi, vc=vc: e.dma_start(out=Cp[h, vc * 128:(vc + 1) * 128, :], in_=f8[fi][:, 0:512]),
                                    reads=["f8_%d" % fi]))
                P.c("pe", lambda e: e.transpose(out=ps[6][0:4, 0:128], in_=nst[:], identity=identf[:]), reads=["nst", "identf"], writes=["ps6"])
                P.c("dve", lambda e: e.tensor_copy(out=small[0:4, 8:8 + 0 + 1].broadcast_to([4, 1]) if False else nout[0:4, 0:128], in_=ps[6][0:4, 0:128]),
                    reads=["ps6"], writes=["nout"])
                outs.append(P.d("sp", lambda e: e.dma_start(out=np_[h].rearrange("(kc k) -> kc k", kc=4), in_=nout[0:4, 0:128]), reads=["nout"], writes=["nout_d"]))
                P.v(["nout"] + ["nout_d"])
                if h < 3:
                    pre_, unit_, post_ = make_sample(h)
                    pre_()
                    posts[h] = post_
                    for j_ in range(16):
                        pending.append(lambda j_=j_, unit_=unit_: unit_(j_))

            _ck("F")
            R3f_ = R3[:].rearrange("p a i c -> p (a i c)")
            W.append(R3f_[:, 10240:10240 + 8192].rearrange("p (k n) -> p k n", k=KC))
            W.append(ARENA[:].rearrange("p (k n) -> p k n", k=KC))
            P.v(["W2"] + [r3k(a_, i_) for a_ in (2, 3) for i_ in range(NT + 1)])
            P.v(["W3", "cqT", ("ot", 0), ("ot", 1), ("KTs", 0), ("KTs", 1)] + KTpk + Vpk)
            NW[0] = 4
            MG = R3[:, 0:2, :, :].rearrange("p a i c -> p (a i c)").bitcast(F32)[:, 0:NT * 512].rearrange("p (i c) -> p i c", c=512)
            P.v([r3k(a_, i_) for a_ in range(4) for i_ in range(NT + 1)] + [("MG", i_) for i_ in range(NT)])
            br = [(wbp, 0, 8, "ga"), (wbm, 8, 16, "gb"), (wbc, 24, 8, "gc")]
            for db in range(4):
                for bidx, (wsrc, kc0, nk, gname) in enumerate(br):
                    sw = load_w(wsrc, 0, nk, db * 512, 512)
                    sg = load_w(w_in, 0, KC, OFF[gname] + db * 512, 512)
                    for i in range(NT):
                        bi = alloc_b()
                        yt = b4[bi]
                        P.d("sp", lambda e, yt=yt, i=i, kc0=kc0, nk=nk: e.dma_start(out=yt[:, 0:nk * 128], in_=yin[i][:, kc0 * 128:(kc0 + nk) * 128]),
                            reads=[("yin", i, kc0 + a) for a in (range(0, nk, 4) if bidx == 1 else [0])], writes=["b4_%d" % bi])
                        gbank = rr("p", 4)
                        mm_group(gbank, [hT[:, k, i * 128:(i + 1) * 128] for k in range(KC)], [W[sg][:, k, :] for k in range(KC)], 512,
                                 reads=hTk(i) + ["W%d" % sg])
                        bs = alloc_b()
                        P.c("act", lambda e, gbank=gbank, bs=bs: e.activation(out=b4[bs][:, 0:512], in_=ps[gbank][:], func=AF.Sigmoid),
                            reads=["ps%d" % gbank], writes=["b4_%d" % bs])
                        ybank = rr("p", 4)
                        mm_group(ybank, [yt[:, k * 128:(k + 1) * 128] for k in range(nk)], [W[sw][:, k, :] for k in range(nk)], 512,
                                 reads=["b4_%d" % bi, "W%d" % sw])
                        if bidx == 0:
                            P.c("dve", lambda e, ybank=ybank, bs=bs, i=i: e.tensor_tensor(out=MG[:, i, :], in0=ps[ybank][:], in1=b4[bs][:, 0:512], op=ALU.mult),
                                reads=["ps%d" % ybank, "b4_%d" % bs], writes=[("MG", i)])
                        else:
                            bt2 = alloc_b()
                            P.c("dve", lambda e, ybank=ybank, bs=bs, bt2=bt2: e.tensor_tensor(out=b4[bt2][:, 0:1024].bitcast(F32), in0=ps[ybank][:],
                                                                                             in1=b4[bs][:, 0:512], op=ALU.mult),
                                reads=["ps%d" % ybank, "b4_%d" % bs], writes=["b4_%d" % bt2])
                            P.c("dve", lambda e, bt2=bt2, i=i: e.tensor_tensor(out=MG[:, i, :], in0=MG[:, i, :], in1=b4[bt2][:, 0:1024].bitcast(F32), op=ALU.add),
                                reads=["b4_%d" % bt2, ("MG", i)], writes=[("MG", i)])
                for i in range(NT):
                    bm = alloc_b()
                    P.c("act", lambda e, bm=bm, i=i: e.copy(out=b4[bm][:, 0:512], in_=MG[:, i, :]), reads=[("MG", i)], writes=["b4_%d" % bm])
                    bank = 4 + rr("q", 2)
                    transposes(bank, [b4[bm][:, c * 128:(c + 1) * 128] for c in range(4)], identb, reads=["b4_%d" % bm, "identb"])
                    P.c("act", lambda e, bank=bank, bm=bm: e.copy(out=b4[bm][:, 512:1024], in_=psb[bank][:, 0:512]), reads=["ps%d" % bank], writes=[("b4m", bm)])
                    P.d("sp", lambda e, bm=bm, i=i, db=db: e.dma_start(out=mgd[i][:, db * 512:(db + 1) * 512], in_=b4[bm][:, 512:1024]),
                        reads=[("b4m", bm)], writes=[("mgd", i, db), "b4_%d" % bm])

            _ck("H")
            P.d("sp", lambda e: e.dma_start(out=gbc[:], in_=g_post.partition_broadcast(128)), writes=["gbc_pre", "gbc_mem", "gbc_post", "Cst"])
            R3f = R3[:].rearrange("p a i c -> p (a i c)")
            wo = [W[0], W[1], R3f[:, 10240:10240 + 8192].rearrange("p (k n) -> p k n", k=KC), None]
            wok = [["W0"], ["W1"], ["W2"], ["R3lo"]]
            P.v(["W2"] + [r3k(a_, i_) for a_ in (2, 3) for i_ in range(NT + 1)])
            ld("pool", wo[2], w_out[:, 1024:1536].rearrange("(kc p) n -> p kc n", p=128), ["W2"])
            P.v(["R3lo"] + [r3k(a_, i_) for a_ in (0, 1) for i_ in range(NT + 1)] + [("MG", i_) for i_ in range(NT)])
            wo[3] = R3f[:, 0:8192].rearrange("p (k n) -> p k n", k=KC)
            ld("pool", wo[3], w_out[:, 1536:2048].rearrange("(kc p) n -> p kc n", p=128), ["R3lo"])
            for cb in range(2):
                ld("pool", W[cb][:], w_out[:, cb * 512:(cb + 1) * 512].rearrange("(kc p) n -> p kc n", p=128), ["W%d" % cb])
            P.v(["W3"])
            def issue_i(i_):
                bi_ = alloc_b()
                P.d("sp", lambda e: e.dma_start(out=b4[bi_][:], in_=mgd[i_]), reads=[("mgd", i_, d_) for d_ in range(4)], writes=["b4_%d" % bi_])
                fx_ = alloc_f()
                P.d("sp", lambda e: e.dma_start(out=f8[fx_][:], in_=xm[i_ * 128:(i_ + 1) * 128, :]), writes=["f8_%d" % fx_])
                return bi_, fx_
            nxt_i = issue_i(0)
            for i in range(NT):
                bi, fx = nxt_i
                xr = f8[fx]
                bj = alloc_b()
                si = rr("t", 3)
                sk = "st%d" % si
                banks = [(i % 2) * 4 + cb for cb in range(4)]
                for cb in range(4):
                    mm_group(banks[cb], [b4[bi][:, k * 128:(k + 1) * 128] for k in range(KC)], [wo[cb][:, k, :] for k in range(KC)], 512,
                             reads=["b4_%d" % bi] + wok[cb])
                    P.c("act", lambda e: e.activation(out=b4[bj][:, 0:512], in_=ps[banks[cb]][:], func=AF.Square, accum_out=msum[:, i, cb:cb + 1]),
                        reads=["ps%d" % banks[cb]], writes=["b4_%d" % bj, ("msum", i, cb)])
                P.c("dve", lambda e: e.tensor_reduce(out=st[:, si, 0:1], in_=msum[:, i, :], axis=AX.X, op=ALU.add),
                    reads=[("msum", i, c) for c in range(4)], writes=[sk])
                P.c("act", lambda e: e.activation(out=st[:, si, 1:2], in_=st[:, si, 0:1], func=AF.Sqrt, scale=1.0 / D, bias=EPS), reads=[sk], writes=[sk])
                P.c("dve", lambda e: e.reciprocal(out=st[:, si, 2:3], in_=st[:, si, 1:2]), reads=[sk], writes=[sk])
                for hf in range(2):
                    bt_ = alloc_b()
                    tmp = b4[bt_][:].bitcast(F32)
                    for q2 in range(2):
                        cb = hf * 2 + q2
                        P.c("dve", lambda e: e.tensor_tensor(out=tmp[:, q2 * 512:(q2 + 1) * 512], in0=ps[banks[cb]][:], in1=gbc[:, cb * 512:(cb + 1) * 512],
                                                             op=ALU.mult), reads=["ps%d" % banks[cb], "gbc_post"], writes=[("b4q", bt_, q2)])
                    P.c("dve", lambda e: e.scalar_tensor_tensor(out=xr[:, hf * 1024:(hf + 1) * 1024], in0=tmp, scalar=st[:, si, 2:3],
                                                                in1=xr[:, hf * 1024:(hf + 1) * 1024], op0=ALU.mult, op1=ALU.add),
                        reads=[("b4q", bt_, 0), ("b4q", bt_, 1), sk, "f8_%d" % fx], writes=[("f8c", fx, hf)])
                if i + 1 < NT:
                    nxt_i = issue_i(i + 1)
                outs.append(P.d("sp", lambda e: e.dma_start(out=y[i * 128:(i + 1) * 128, :], in_=xr[:]), reads=[("f8c", fx, 0), ("f8c", fx, 1)]))
        except _Stop:
            pass
        P.final(outs)
        P.emit()
    return nc


def _consts(s):
    t = np.arange(128)
    S, T = np.meshgrid(t, t, indexing="ij")
    uf = (S <= T).astype(np.float32)
    same = (S // 8 == T // 8)
    ub = (uf * same).astype(np.float32)
    of_ = np.ones((128, 128), np.float32)
    ob = same.astype(np.float32)
    band = np.zeros((128, 16, 128), np.float32)
    bandb = np.zeros((120, 8, 128), np.float32)
    eye = np.eye(128, dtype=np.float32)
    for g, w in enumerate((2, 4, 8, 16)):
        cur = ((S <= T) & (S > T - w)).astype(np.float32) / w - eye
        cnt = np.minimum(w, T + 1).astype(np.float32)
        cur0 = ((S <= T) & (S > T - w)).astype(np.float32) / cnt - eye
        band[:, 0 * 4 + g, :] = cur0 if s == 0 else cur
        band[:, 1 * 4 + g, :] = cur
        band[:, 2 * 4 + g, :] = ((S - 128) > (T - w)).astype(np.float32) / w
        band[:, 3 * 4 + g, :] = (same & (S <= T) & (S > T - w)).astype(np.float32) / w - eye
        r = np.arange(120)
        R, TT = np.meshgrid(r, t, indexing="ij")
        for c2 in range(2):
            jj = c2 * 8 + R // 15
            ii = R % 15
            bandb[:, g * 2 + c2, :] = ((jj == TT // 8) & ((ii - 15) > (TT % 8 - w))).astype(np.float32) / w
    rowm = (t[:, None] // 8 == np.arange(16)[None, :]).astype(np.float32)
    sel8 = (t[:, None] == 8 * np.arange(16)[None, :]).astype(np.float32)
    blk = np.broadcast_to(rowm.T[None, :, :], (128, 16, 128)).astype(np.float32).copy()
    selc = np.full((128, 1), float(s), np.float32)
    return dict(c_ident=eye, c_uf=uf, c_ub=ub, c_of=of_, c_ob=ob, c_band=band, c_bandb=bandb, c_rowm=rowm, c_sel8=sel8,
                c_blk=blk, c_selc=selc)


def make_in_maps(x_prompt, x_sample, state_pool, state_mlstm_C, state_mlstm_n, state_mlstm_m, cache_mem_k, cache_mem_v, mem_prompt,
                 g_pre, g_post, w_in, b_mlstm_i, b_mlstm_f, w_pool_grp, pool_scale, g_mem, w_mem_kv, w_br_pool, w_br_mlstm, w_br_mem, w_out,
                 cores=range(8)):
    f = lambda a: np.ascontiguousarray(np.asarray(a, dtype=np.float32))
    x_prompt, x_sample = f(x_prompt), f(x_sample)
    shared = dict(w_in=f(w_in[0]), w_kv=f(w_mem_kv[0]), wbp=f(w_br_pool[0]), wbm=f(w_br_mlstm[0]), wbc=f(w_br_mem[0]), w_out=f(w_out[0]),
                  wpg=f(w_pool_grp[0]), g_pre=f(g_pre), g_post=f(g_post), g_mem=f(g_mem), pscale=f(pool_scale),
                  bif=f(np.concatenate([np.asarray(b_mlstm_i), np.asarray(b_mlstm_f)], axis=1)))
    in_maps = []
    for c in cores:
        b, s = c // 2, c % 2
        sl = slice(16 * c, 16 * c + 16)
        xm = np.concatenate([x_prompt[b, 1024 * s:1024 * (s + 1)], x_sample[sl].reshape(128, D)], axis=0)
        xp = x_prompt[b, 0:1024] if s == 1 else np.zeros((1024, D), np.float32)
        m = dict(shared)
        m.update(xm=f(xm), xp=f(xp), memx=f(mem_prompt[b]), spool=f(state_pool[0, sl]), sC=f(state_mlstm_C[0, sl]),
                 sn=f(state_mlstm_n[0, sl]), sm=f(state_mlstm_m[0, sl]), ck=f(np.asarray(cache_mem_k)[0, sl].reshape(16, 256, 1024)),
                 cv=f(np.asarray(cache_mem_v)[0, sl].reshape(16, 256, 1024)))
        m.update(_consts(s))
        in_maps.append(m)
    return in_maps


_NC = None


def kernel(x_prompt, x_sample, state_pool, state_mlstm_C, state_mlstm_n, state_mlstm_m, cache_mem_k, cache_mem_v, mem_prompt,
           g_pre, g_post, w_in, b_mlstm_i, b_mlstm_f, w_pool_grp, pool_scale, g_mem, w_mem_kv, w_br_pool, w_br_mlstm, w_br_mem, w_out):
    global _NC
    in_maps = make_in_maps(**locals())
    if _NC is None:
        _NC = build()
    res = run_bass_kernel_spmd(_NC, in_maps, core_ids=list(range(8))).results
    y_p = np.zeros((4, 2048, D), np.float32); y_s = np.zeros((128, 8, D), np.float32)
    pool_p = np.zeros((1, 4, 15, 1024), np.float32); C_p = np.zeros((1, 4, 4, 512, 512), np.float32)
    n_p = np.zeros((1, 4, 4, 512), np.float32); m_p = np.zeros((1, 4, 4), np.float32)
    mk_p = np.zeros((1, 4, 256, 4, 256), np.float32); mv_p = np.zeros((1, 4, 256, 4, 256), np.float32)
    pool_s = np.zeros((1, 128, 15, 1024), np.float32); C_s = np.zeros((1, 128, 4, 512, 512), np.float32)
    n_s = np.zeros((1, 128, 4, 512), np.float32); m_s = np.zeros((1, 128, 4), np.float32)
    for c in range(8):
        b, s = c // 2, c % 2
        sl = slice(16 * c, 16 * c + 16)
        r = res[c]
        y_p[b, 1024 * s:1024 * (s + 1)] = r["y"][0:1024]
        y_s[sl] = r["y"][1024:1152].reshape(16, 8, D)
        pool_s[0, sl] = r["pools"]; C_s[0, sl] = r["Cs"]; n_s[0, sl] = r["ns"]; m_s[0, sl] = r["ms"]
        if s == 1:
            pool_p[0, b] = r["poolp"]; C_p[0, b] = r["Cp"]; n_p[0, b] = r["np_"]; m_p[0, b] = r["mp"][0]
        else:
            mk_p[0, b] = r["mk"].reshape(256, 4, 256); mv_p[0, b] = r["mv"].reshape(256, 4, 256)
    return (y_p, y_s, pool_p, C_p, n_p, m_p, mk_p, mv_p, pool_s, C_s, n_s, m_s)
```
